# Optimizing a Trainium2 kernel written in Bass

```python
import math
import jax, jax.numpy as jnp
from jax import lax
import numpy as np


D_MODEL = 1024
BATCH = 8
SEQ = 4096
DEPTH = 1

ATTN_Q_HEADS = 8
ATTN_KV_HEADS = 2
ATTN_HEAD_DIM = 64
WINDOW = 128
ATTN_BLOCK = 128
ROPE_THETA = 10000.0
GLA_HEADS = 4
GLA_KEY_DIM = 64
GLA_VAL_DIM = 128
GLA_GATE_RANK = 16
GLA_GATE_NORM = 16.0
GLA_CHUNK = 64
ATTN_WIDTH = ATTN_Q_HEADS * ATTN_HEAD_DIM
GLA_WIDTH = GLA_HEADS * GLA_VAL_DIM
MIX_WIDTH = ATTN_WIDTH + GLA_WIDTH
IN_SIZES = (ATTN_Q_HEADS * ATTN_HEAD_DIM, ATTN_KV_HEADS * ATTN_HEAD_DIM, ATTN_KV_HEADS * ATTN_HEAD_DIM,
            GLA_HEADS * GLA_KEY_DIM, GLA_HEADS * GLA_KEY_DIM, GLA_WIDTH, GLA_WIDTH, GLA_GATE_RANK)
IN_WIDTH = sum(IN_SIZES)
N_EXPERTS = 256
TOP_K = 8
N_GROUPS = 8
TOPK_GROUPS = 4
EXPERT_HIDDEN = 256
SHARED_HIDDEN = 256
ROUTED_SCALE = 2.5
EXPERT_BLOCK = 128
DEEPNORM_ALPHA = (2 * DEPTH) ** 0.25
DEEPNORM_BETA = (8 * DEPTH) ** -0.25
LN_EPS = 1e-5
NEG_INF = -1e30

kernel_name = 'hybrid_swa_sink_gla_moe_deepnorm_adaln'


def layer_norm_plain(x):
    x32 = x.astype(jnp.float32)
    mu = jnp.mean(x32, axis=-1, keepdims=True)
    var = jnp.mean(jnp.square(x32 - mu), axis=-1, keepdims=True)
    return (x32 - mu) * lax.rsqrt(var + LN_EPS)


def layer_norm(x, g, b):
    return (layer_norm_plain(x) * g.astype(jnp.float32) + b.astype(jnp.float32)).astype(x.dtype)


def modulate(x, shift, scale):
    y = layer_norm_plain(x) * (1.0 + scale.astype(jnp.float32)) + shift.astype(jnp.float32)
    return y.astype(x.dtype)


def rope(t, positions):
    half = t.shape[-1] // 2
    inv_freq = ROPE_THETA ** (-jnp.arange(half, dtype=jnp.float32) / half)
    ang = positions.astype(jnp.float32)[..., None] * inv_freq
    cos = jnp.cos(ang)[:, :, None, :]
    sin = jnp.sin(ang)[:, :, None, :]
    t32 = t.astype(jnp.float32)
    t1, t2 = t32[..., :half], t32[..., half:]
    return jnp.concatenate([t1 * cos - t2 * sin, t2 * cos + t1 * sin], axis=-1)


def sliding_window_sink_attention(q, k, v, sinks):
    B, S, HQ, Dh = q.shape
    HKV = k.shape[2]
    G = HQ // HKV
    nb = S // ATTN_BLOCK
    qb = q.reshape(B, nb, ATTN_BLOCK, HKV, G, Dh)

    def with_prev(t):
        prev = jnp.pad(t[:, :-1], ((0, 0), (1, 0), (0, 0), (0, 0), (0, 0)))
        return jnp.concatenate([prev, t], axis=2)

    kw = with_prev(k.astype(jnp.float32).reshape(B, nb, ATTN_BLOCK, HKV, Dh))
    vw = with_prev(v.astype(jnp.float32).reshape(B, nb, ATTN_BLOCK, HKV, Dh))
    s = jnp.einsum('bnqhgd,bnkhd->bnhgqk', qb, kw) * (Dh ** -0.5)
    qi = jnp.arange(ATTN_BLOCK)[:, None]
    kj = jnp.arange(2 * ATTN_BLOCK)[None, :]
    dist = qi + ATTN_BLOCK - kj
    band = (dist >= 0) & (dist < WINDOW)
    key_abs = (jnp.arange(nb)[:, None] - 1) * ATTN_BLOCK + jnp.arange(2 * ATTN_BLOCK)[None, :]
    valid = band[None] & (key_abs >= 0)[:, None, :]
    s = jnp.where(valid[None, :, None, None], s, NEG_INF)
    sink = jnp.broadcast_to(sinks.astype(jnp.float32).reshape(1, 1, HKV, G, 1, 1), s.shape[:-1] + (1,))
    p = jax.nn.softmax(jnp.concatenate([s, sink], axis=-1), axis=-1)[..., :-1]
    o = jnp.einsum('bnhgqk,bnkhd->bnqhgd', p, vw)
    return o.reshape(B, S, HQ * Dh)


def gla_chunked(q, k, v, log_g):
    B, S, H, dk = q.shape
    dv = v.shape[-1]
    n = S // GLA_CHUNK
    C = GLA_CHUNK
    qc = q.astype(jnp.float32).reshape(B, n, C, H, dk) * (dk ** -0.5)
    kc = k.astype(jnp.float32).reshape(B, n, C, H, dk)
    vc = v.astype(jnp.float32).reshape(B, n, C, H, dv)
    b = jnp.cumsum(log_g.reshape(B, n, C, H, dk), axis=2)
    b_last = b[:, :, -1]
    q_in = qc * jnp.exp(b)
    k_in = kc * jnp.exp(-b)
    k_out = kc * jnp.exp(b_last[:, :, None] - b)
    causal = jnp.tril(jnp.ones((C, C), dtype=bool))
    a = jnp.einsum('bnchd,bnshd->bnhcs', q_in, k_in)
    a = jnp.where(causal, a, 0.0)
    o_intra = jnp.einsum('bnhcs,bnshv->bnchv', a, vc)
    u = jnp.einsum('bnshd,bnshv->bnhdv', k_out, vc)
    decay = jnp.exp(b_last)

    def step(state, inp):
        dec, inc = inp
        return dec[..., None] * state + inc, state

    _, s_prev = lax.scan(step, jnp.zeros((B, H, dk, dv), jnp.float32),
                         (jnp.moveaxis(decay, 1, 0), jnp.moveaxis(u, 1, 0)))
    s_prev = jnp.moveaxis(s_prev, 0, 1)
    o_inter = jnp.einsum('bnchd,bnhdv->bnchv', q_in, s_prev)
    return (o_intra + o_inter).reshape(B, S, H, dv)


def token_mixer(h, positions, w_in, b_in, attn_sinks, w_gk2, b_gk2, gla_norm_g, w_o, b_o):
    B, S, _ = h.shape
    proj = h @ w_in + b_in
    q_a, k_a, v_a, q_l, k_l, v_l, g_l, gk_lo = jnp.split(proj, np.cumsum(IN_SIZES)[:-1].tolist(), axis=-1)
    q_a = rope(q_a.reshape(B, S, ATTN_Q_HEADS, ATTN_HEAD_DIM), positions)
    k_a = rope(k_a.reshape(B, S, ATTN_KV_HEADS, ATTN_HEAD_DIM), positions)
    v_a = v_a.reshape(B, S, ATTN_KV_HEADS, ATTN_HEAD_DIM)
    o_attn = sliding_window_sink_attention(q_a, k_a, v_a, attn_sinks)
    log_g = jax.nn.log_sigmoid((gk_lo @ w_gk2 + b_gk2).astype(jnp.float32)) / GLA_GATE_NORM
    o_gla = gla_chunked(q_l.reshape(B, S, GLA_HEADS, GLA_KEY_DIM),
                        k_l.reshape(B, S, GLA_HEADS, GLA_KEY_DIM),
                        v_l.reshape(B, S, GLA_HEADS, GLA_VAL_DIM),
                        log_g.reshape(B, S, GLA_HEADS, GLA_KEY_DIM))
    o_gla = o_gla * lax.rsqrt(jnp.mean(jnp.square(o_gla), axis=-1, keepdims=True) + LN_EPS) * gla_norm_g.astype(jnp.float32)
    o_gla = o_gla.reshape(B, S, GLA_WIDTH) * jax.nn.silu(g_l.astype(jnp.float32))
    mixed = jnp.concatenate([o_attn, o_gla], axis=-1).astype(h.dtype)
    return mixed @ w_o + b_o


def swiglu(x, w_gate, w_up, w_down):
    return (jax.nn.silu(x @ w_gate) * (x @ w_up)) @ w_down


def moe_ffn(h, w_router, router_bias, w_exp_gate, w_exp_up, w_exp_down, w_sh_gate, w_sh_up, w_sh_down):
    B, S, D = h.shape
    N = B * S
    xt = h.reshape(N, D)
    scores = jax.nn.sigmoid((xt @ w_router).astype(jnp.float32))
    biased = scores + router_bias.astype(jnp.float32)
    grp = biased.reshape(N, N_GROUPS, N_EXPERTS // N_GROUPS)
    grp_score = jnp.sum(lax.top_k(grp, 2)[0], axis=-1)
    _, top_g = lax.top_k(grp_score, TOPK_GROUPS)
    gmask = jnp.any(top_g[..., None] == jnp.arange(N_GROUPS), axis=1)
    emask = jnp.repeat(gmask, N_EXPERTS // N_GROUPS, axis=-1)
    _, top_idx = lax.top_k(jnp.where(emask, biased, NEG_INF), TOP_K)
    top_w = jnp.take_along_axis(scores, top_idx, axis=-1)
    top_w = top_w / jnp.sum(top_w, axis=-1, keepdims=True) * ROUTED_SCALE
    A = N * TOP_K
    flat_e = top_idx.reshape(A)
    flat_w = top_w.reshape(A)
    flat_t = jnp.arange(A, dtype=jnp.int32) // TOP_K
    order = jnp.argsort(flat_e, stable=True)
    e_s, t_s, w_s = flat_e[order], flat_t[order], flat_w[order]
    counts = jnp.bincount(flat_e, length=N_EXPERTS)
    starts = jnp.cumsum(counts) - counts
    padded = (counts + EXPERT_BLOCK - 1) // EXPERT_BLOCK * EXPERT_BLOCK
    pends = jnp.cumsum(padded)
    pstarts = pends - padded
    dest = pstarts[e_s] + jnp.arange(A, dtype=jnp.int32) - starts[e_s]
    P = A + N_EXPERTS * EXPERT_BLOCK
    n_blk = P // EXPERT_BLOCK
    slot_tok = jnp.full((P,), N, jnp.int32).at[dest].set(t_s)
    slot_w = jnp.zeros((P,), jnp.float32).at[dest].set(w_s)
    blk_exp = jnp.minimum(jnp.searchsorted(pends, jnp.arange(n_blk) * EXPERT_BLOCK, side='right'), N_EXPERTS - 1)
    x_pad = jnp.concatenate([xt, jnp.zeros((1, D), xt.dtype)], axis=0)

    def expert_block(args):
        tok, e, w = args
        yb = swiglu(x_pad[tok], w_exp_gate[e], w_exp_up[e], w_exp_down[e])
        return yb.astype(jnp.float32) * w[:, None]

    y = lax.map(expert_block, (slot_tok.reshape(n_blk, EXPERT_BLOCK), blk_exp, slot_w.reshape(n_blk, EXPERT_BLOCK)))
    routed = jax.ops.segment_sum(y.reshape(P, D), slot_tok, num_segments=N + 1)[:N]
    shared = swiglu(xt, w_sh_gate, w_sh_up, w_sh_down).astype(jnp.float32)
    return (shared + routed).astype(h.dtype).reshape(B, S, D)


def setup_inputs(seed: int = 0) -> dict:
    key = jax.random.key(seed)
    ks = jax.random.split(key, 28)
    f32 = jnp.float32
    L, D, E, H = DEPTH, D_MODEL, N_EXPERTS, EXPERT_HIDDEN

    def nrm(k, shape, scale):
        return jax.random.normal(k, shape, f32) * scale

    offs = np.cumsum((0,) + IN_SIZES)
    col_scale = np.ones((IN_WIDTH,), np.float32)
    col_scale[offs[2]:offs[3]] = DEEPNORM_BETA
    col_scale[offs[5]:offs[6]] = DEEPNORM_BETA
    positions = (jax.random.randint(ks[2], (BATCH, 1), 0, 1024, jnp.int32)
                 + jnp.arange(SEQ, dtype=jnp.int32)[None, :])
    return {
        'x': nrm(ks[0], (BATCH, SEQ, D), 1.0),
        'c': nrm(ks[1], (BATCH, D), 1.0),
        'positions': positions,
        'w_ada': nrm(ks[3], (L, D, 6 * D), 0.2 * D ** -0.5),
        'b_ada': nrm(ks[4], (L, 6 * D), 0.02),
        'w_in': nrm(ks[5], (L, D, IN_WIDTH), D ** -0.5) * jnp.asarray(col_scale),
        'b_in': nrm(ks[6], (L, IN_WIDTH), 0.02),
        'attn_sinks': nrm(ks[7], (L, ATTN_Q_HEADS), 0.5),
        'w_gk2': nrm(ks[8], (L, GLA_GATE_RANK, GLA_HEADS * GLA_KEY_DIM), GLA_GATE_RANK ** -0.5),
        'b_gk2': nrm(ks[9], (L, GLA_HEADS * GLA_KEY_DIM), 0.1),
        'gla_norm_g': 1.0 + nrm(ks[10], (L, GLA_VAL_DIM), 0.02),
        'w_o': nrm(ks[11], (L, MIX_WIDTH, D), MIX_WIDTH ** -0.5 * DEEPNORM_BETA),
        'b_o': nrm(ks[12], (L, D), 0.02),
        'ln1_g': 1.0 + nrm(ks[13], (L, D), 0.02),
        'ln1_b': nrm(ks[14], (L, D), 0.02),
        'w_router': nrm(ks[15], (L, D, E), D ** -0.5),
        'router_bias': nrm(ks[16], (L, E), 0.01),
        'w_exp_gate': nrm(ks[17], (L, E, D, H), D ** -0.5),
        'w_exp_up': nrm(ks[18], (L, E, D, H), D ** -0.5 * DEEPNORM_BETA),
        'w_exp_down': nrm(ks[19], (L, E, H, D), H ** -0.5 * DEEPNORM_BETA),
        'w_sh_gate': nrm(ks[20], (L, D, SHARED_HIDDEN), D ** -0.5),
        'w_sh_up': nrm(ks[21], (L, D, SHARED_HIDDEN), D ** -0.5 * DEEPNORM_BETA),
        'w_sh_down': nrm(ks[22], (L, SHARED_HIDDEN, D), SHARED_HIDDEN ** -0.5 * DEEPNORM_BETA),
        'ln2_g': 1.0 + nrm(ks[23], (L, D), 0.02),
        'ln2_b': nrm(ks[24], (L, D), 0.02),
    }


def reference(x, c, positions, w_ada, b_ada, w_in, b_in, attn_sinks, w_gk2, b_gk2, gla_norm_g, w_o, b_o,
              ln1_g, ln1_b, w_router, router_bias, w_exp_gate, w_exp_up, w_exp_down,
              w_sh_gate, w_sh_up, w_sh_down, ln2_g, ln2_b):
    c_act = jax.nn.silu(c)
    for l in range(DEPTH):
        mod = (c_act @ w_ada[l] + b_ada[l])[:, None, :]
        sh1, sc1, g1, sh2, sc2, g2 = jnp.split(mod, 6, axis=-1)
        h = modulate(x, sh1, sc1)
        y = token_mixer(h, positions, w_in[l], b_in[l], attn_sinks[l], w_gk2[l], b_gk2[l],
                        gla_norm_g[l], w_o[l], b_o[l])
        x = layer_norm(DEEPNORM_ALPHA * x + (1.0 + g1) * y, ln1_g[l], ln1_b[l])
        h = modulate(x, sh2, sc2)
        y = moe_ffn(h, w_router[l], router_bias[l], w_exp_gate[l], w_exp_up[l], w_exp_down[l],
                    w_sh_gate[l], w_sh_up[l], w_sh_down[l])
        x = layer_norm(DEEPNORM_ALPHA * x + (1.0 + g2) * y, ln2_g[l], ln2_b[l])
    return x
```

```python
import os
import threading
import numpy as np
from contextlib import ExitStack
import concourse.bass as bass
import concourse.mybir as mybir
from concourse.bass_utils import run_bass_kernel_spmd

F32 = mybir.dt.float32
BF16 = mybir.dt.bfloat16
I32 = mybir.dt.int32
U32 = mybir.dt.uint32
AF = mybir.ActivationFunctionType
ALU = mybir.AluOpType

S = 4096
D = 1024
NT = S // 128
INW = 2320
NE = 256
NBLK = 512
ALPHA = 2.0 ** 0.25
EPS = 1e-5
BIG = 1.0e30
NDS = 40
NSUB = 8

C_ID, C_TRI, C_BO, C_LS, C_ON, C_MP, C_MC = 0, 128, 256, 384, 512, 640, 768
C_IOTA = 896
C_BV = 1152
C_INVF = 1156
C_PI = 1188
NCONST = 1189


class Buf:
    def __init__(self, t):
        self.t = t
        self.w = {}
        self.r = {}

    def __getitem__(self, k):
        return self.t[k]


class Acc:
    def __init__(self, b):
        self.b = b


class Sch:
    def __init__(self, nc, st):
        self.nc = nc
        self.eng = dict(pe=nc.tensor, dve=nc.vector, act=nc.scalar, pool=nc.gpsimd, sp=nc.sync)
        self.csem = {k: st.enter_context(nc.semaphore("c_" + k)) for k in ["pe", "dve", "act", "pool"]}
        self.ccnt = {k: 0 for k in self.csem}
        self.dsems = [st.enter_context(nc.semaphore("d%d" % i)) for i in range(NDS)]
        self.dval = [0] * NDS
        self.dnext = 0
        self.waited = {k: {} for k in self.eng}
        self.n_inst = 0
        self.il = None

    def _wait(self, e, ev):
        sem, val, src = ev
        key = id(sem)
        if self.waited[e].get(key, 0) >= val:
            return
        self.eng[e].wait_ge(sem, val)
        self.waited[e][key] = val

    def _deps(self, e, R, W, acc):
        for b in R:
            for ev in b.w.values():
                if not (e == "pe" and ev[2] == "pe"):
                    self._wait(e, ev)
        for b in W:
            a_ = acc
            if isinstance(b, Acc):
                a_, b = True, b.b
            if not a_:
                for ev in b.w.values():
                    if not (e == "pe" and ev[2] == "pe"):
                        self._wait(e, ev)
            for ev in b.r.values():
                if not (e == "pe" and ev[2] == "pe"):
                    self._wait(e, ev)

    def _upd(self, ev, R, W, acc):
        key = id(ev[0])
        for b in R:
            b.r[key] = ev
        for b in W:
            a_ = acc
            if isinstance(b, Acc):
                a_, b = True, b.b
            if a_:
                b.w[key] = ev
            else:
                b.w = {key: ev}
            b.r = {}

    def op(self, e, fn, R=(), W=(), acc=False):
        self._deps(e, R, W, acc)
        inst = fn()
        self.ccnt[e] += 1
        inst.then_inc(self.csem[e], 1)
        self._upd((self.csem[e], self.ccnt[e], e), R, W, acc)
        self.n_inst += 1
        self._yield()

    def dma(self, q, fn, R=(), W=(), acc=False):
        i = self.dnext
        self.dnext = (i + 1) % NDS
        if self.dval[i] > 0:
            self._wait(q, (self.dsems[i], self.dval[i], "dma"))
        self._deps(q, R, W, acc)
        inst = fn()
        self.dval[i] += 16
        inst.then_inc(self.dsems[i], 16)
        self._upd((self.dsems[i], self.dval[i], "dma"), R, W, acc)
        self.n_inst += 1
        self._yield()

    def barrier(self):
        for e in self.eng:
            for k, sem in self.csem.items():
                if self.ccnt[k] > 0 and not (e == "pe" and k == "pe"):
                    self._wait(e, (sem, self.ccnt[k], k))
            for i, sem in enumerate(self.dsems):
                if self.dval[i] > 0:
                    self._wait(e, (sem, self.dval[i], "dma"))

    def _yield(self):
        il = self.il
        if il is None:
            return
        me = threading.current_thread().name
        names = il["names"]
        k = names.index(me)
        for d in range(1, len(names)):
            o = names[(k + d) % len(names)]
            if not il["done"][o]:
                il["sem"][o].release()
                il["sem"][me].acquire()
                return

    def interleave(self, *fs):
        names = ["S%d" % i for i in range(len(fs))]
        il = dict(names=names, sem={n: threading.Semaphore(0) for n in names}, done={n: False for n in names}, exc=[])
        self.il = il

        def body(name, f):
            il["sem"][name].acquire()
            try:
                f()
            except BaseException as e:
                il["exc"].append(e)
            il["done"][name] = True
            k = names.index(name)
            for d in range(1, len(names)):
                o = names[(k + d) % len(names)]
                if not il["done"][o]:
                    il["sem"][o].release()
                    break

        ths = [threading.Thread(target=body, args=(n, f), name=n) for n, f in zip(names, fs)]
        for t in ths:
            t.start()
        il["sem"][names[0]].release()
        for t in ths:
            t.join()
        self.il = None
        if il["exc"]:
            raise il["exc"][0]

    def finish(self, bufs):
        for b in bufs:
            for ev in b.w.values():
                self._wait("sp", ev)


def build(debug=False):
    nc = bass.Bass("TRN2", target_bir_lowering=False)
    dt_in = lambda n, s, d=F32: nc.dram_tensor(n, s, d, kind="ExternalInput").ap()
    x_d = dt_in("x", [S, D])
    c_d = dt_in("c", [128, 8])
    pos_d = dt_in("pos", [128, NT], I32)
    consts_d = dt_in("consts", [128, NCONST])
    wada_d = dt_in("w_ada", [D, 6 * D])
    bada_d = dt_in("b_ada", [1, 6 * D])
    win_d = dt_in("w_in", [D, INW])
    bin_d = dt_in("b_in", [1, INW])
    bgkT_d = dt_in("b_gklo", [16, 1])
    sinks_d = dt_in("sinks", [1, 8])
    wgk2_d = dt_in("w_gk2", [16, 256])
    bgk2_d = dt_in("b_gk2", [1, 256])
    gnorm_d = dt_in("gnorm", [1, 512])
    wo_d = dt_in("w_o", [D, D])
    bo_d = dt_in("b_o", [1, D])
    ln1g_d = dt_in("ln1_g", [1, D])
    ln1b_d = dt_in("ln1_b", [1, D])
    wr_d = dt_in("w_router", [D, NE])
    rb_d = dt_in("router_bias", [1, NE])
    ne_rows = 128 if (debug and not os.environ.get('K_FULL')) else NE * 128
    weg_d = dt_in("w_exp_gate", [ne_rows, 2048])
    weu_d = dt_in("w_exp_up", [ne_rows, 2048])
    wed_d = dt_in("w_exp_down", [ne_rows, 2048])
    wsg_d = dt_in("w_sh_gate", [D, 256])
    wsu_d = dt_in("w_sh_up", [D, 256])
    wsd_d = dt_in("w_sh_down", [256, D])
    ln2g_d = dt_in("ln2_g", [1, D])
    ln2b_d = dt_in("ln2_b", [1, D])
    out_d = nc.dram_tensor("out", [S, D], F32, kind="ExternalOutput").ap()
    dbg_d = nc.dram_tensor("dbg", [8, 128, D], F32, kind="ExternalOutput").ap() if debug else None
    dbg16_d = nc.dram_tensor("dbg16", [4, 128, D], BF16, kind="ExternalOutput").ap() if debug else None
    z_s = nc.dram_tensor("z_s", [S, D], F32, kind="ExternalOutput" if debug else "Internal").ap()
    h2_s = nc.dram_tensor("h2_s", [S, D], BF16, kind="ExternalOutput" if debug else "Internal").ap()
    pf_s = nc.dram_tensor("pf_s", [S, NE], F32, kind="Internal").ap()
    xdisp = nc.dram_tensor("xdisp", [NBLK * 128, D], BF16, kind="Internal").ap()
    ydisp = nc.dram_tensor("ydisp", [NBLK * 128, D], BF16, kind="Internal").ap()

    with ExitStack() as st:
        sc = Sch(nc, st)
        V, A, P, PE = nc.vector, nc.scalar, nc.gpsimd, nc.tensor

        def sb(stack, name, shape, dt=F32):
            return Buf(stack.enter_context(nc.sbuf_tensor(name, shape, dt)))

        def ps(stack, name, shape, dt=F32):
            return Buf(stack.enter_context(nc.psum_tensor(name, shape, dt)))

        B_pf = Buf(None); B_out = Buf(None); B_z = Buf(None); B_h2s = Buf(None); B_xd = Buf(None); B_yd = Buf(None); B_dbg = Buf(None)

        cst = sb(st, "cst", [128, NCONST])
        cb16 = sb(st, "cb16", [128, 896], BF16)
        modv = sb(st, "modv", [128, 6 * D])
        idxf_t = sb(st, "idxf_t", [128, NT, 8])
        w_t = sb(st, "w_t", [128, NT, 8])
        cntb = sb(st, "cntb", [128, NE])
        st_ps = ExitStack()
        ps_tr = ps(st_ps, "ps_tr", [128, 1024], BF16)
        pm = [ps(st_ps, "pm%d" % i, [128, 512]) for i in range(6)]
        ps_tr2 = ps(st_ps, "ps_tr2", [128, 1024], BF16)

        ident = lambda: cst[:, C_ID:C_ID + 128]
        id16 = lambda: cb16[:, 0:128]

        dq = ["sp", "act"]
        sc.dma("sp", lambda: nc.sync.dma_start(out=cst[:], in_=consts_d[:, :]), W=[cst])
        sc.op("dve", lambda: V.tensor_copy(cb16[:], cst[:, 0:896]), R=[cst], W=[cb16])
        sc.op("pool", lambda: P.memset(cntb[:], 0.0), W=[cntb])

        def ln_stats(stk_tiles, src, R):
            stats, mv, rstd, nmr = stk_tiles
            sc.op("dve", lambda: V.bn_stats(stats[:, 0:6], src[:, 0:512]), R=R, W=[stats])
            sc.op("dve", lambda: V.bn_stats(stats[:, 6:12], src[:, 512:1024]), R=R, W=[stats], acc=True)
            sc.op("dve", lambda: V.bn_aggr(mv[:], stats[:]), R=[stats], W=[mv])
            sc.op("act", lambda: A.activation(rstd[:], mv[:, 1:2], AF.Sqrt, bias=EPS, scale=1.0), R=[mv], W=[rstd])
            sc.op("dve", lambda: V.reciprocal(rstd[:], rstd[:]), R=[rstd], W=[rstd])
            sc.op("dve", lambda: V.tensor_scalar(nmr[:], mv[:, 0:1], rstd[:, 0:1], -1.0, ALU.mult, ALU.mult), R=[mv, rstd], W=[nmr])
            return mv, rstd

        with ExitStack() as s0:
            c8 = sb(s0, "c8", [128, 8]); ca8 = sb(s0, "ca8", [128, 8])
            cbT = sb(s0, "cbT", [128, 8, 128])
            wst = [sb(s0, "wada%d" % i, [128, 8, 512]) for i in range(2)]
            bada = sb(s0, "bada", [128, 6 * D])
            sc.dma("sp", lambda: nc.sync.dma_start(out=c8[:], in_=c_d[:, :]), W=[c8])
            sc.dma("act", lambda: A.dma_start(out=bada[:], in_=bada_d.partition_broadcast(128)), W=[bada])
            sc.op("act", lambda: A.activation(ca8[:], c8[:], AF.Silu), R=[c8], W=[ca8])
            for j in range(8):
                sc.op("dve", lambda j=j: V.tensor_scalar(cbT[:, j, :], cst[:, C_ON:C_ON + 128], ca8[:, j:j + 1], None,
                                                         ALU.mult), R=[ca8, cst], W=[cbT], acc=(j > 0))
            wv = wada_d.rearrange("(p j) n -> p j n", j=8)
            for n in range(12):
                w = wst[n % 2]
                sc.dma(dq[n % 2], lambda n=n, w=w: sc.eng[dq[n % 2]].dma_start(out=w[:], in_=wv[:, :, n * 512:(n + 1) * 512]), W=[w])
                p = pm[n % 2]
                for j in range(8):
                    sc.op("pe", lambda j=j, w=w, p=p: PE.matmul(p[:], cbT[:, j, :], w[:, j, :], start=(j == 0), stop=(j == 7)),
                          R=[cbT, w], W=[p], acc=(j > 0))
                sc.op("dve", lambda n=n, p=p: V.tensor_tensor(modv[:, n * 512:(n + 1) * 512], p[:], bada[:, n * 512:(n + 1) * 512],
                                                              ALU.add), R=[p, bada], W=[modv], acc=True)
            for sec in (1, 2, 4, 5):
                sc.op("dve", lambda sec=sec: V.tensor_scalar(modv[:, sec * D:(sec + 1) * D], modv[:, sec * D:(sec + 1) * D], 1.0,
                                                             None, ALU.add), R=[modv], W=[modv])
        sc.barrier()
        SH1, SC1, G1, SH2, SC2, G2 = [lambda k=k: modv[:, k * D:(k + 1) * D] for k in range(6)]

        with ExitStack() as sa:
            win16 = sb(sa, "win16", [128, 8, INW], BF16)
            wo16 = sb(sa, "wo16", [128, 8, D], BF16)
            wr16 = sb(sa, "wr16", [128, 8, NE], BF16)
            wsgu16 = sb(sa, "wsgu16", [128, 8, 512], BF16)
            wsd16 = sb(sa, "wsd16", [128, 2, D], BF16)
            wgk2 = sb(sa, "wgk2", [16, 256]); bgkT = sb(sa, "bgkT", [16, 1])
            binb = sb(sa, "binb", [128, INW], BF16); bgk2b = sb(sa, "bgk2b", [128, 256]); gnb = sb(sa, "gnb", [128, 512])
            bog1 = sb(sa, "bog1", [128, D]); ln1g = sb(sa, "ln1g", [128, D]); ln1b = sb(sa, "ln1b", [128, D])
            rbb = sb(sa, "rbb", [128, NE]); esink = sb(sa, "esink", [128, 8])
            cosT = sb(sa, "cosT", [128, NT, 32], BF16); sinT = sb(sa, "sinT", [128, NT, 32], BF16)
            with ExitStack() as sl:
                stg = [sb(sl, "stg%d" % i, [128, 8, 512]) for i in range(2)]
                k = 0
                win_v = win_d.rearrange("(j p) c -> p j c", p=128)
                jobs = []
                for c0 in range(0, INW, 512):
                    jobs.append((win_v, c0, min(512, INW - c0), win16, c0, 8))
                wo_v = wo_d.rearrange("(j p) c -> p j c", p=128)
                for c0 in range(0, D, 512):
                    jobs.append((wo_v, c0, 512, wo16, c0, 8))
                jobs.append((wr_d.rearrange("(p j) c -> p j c", j=8), 0, 256, wr16, 0, 8))
                jobs.append((wsg_d.rearrange("(p j) c -> p j c", j=8), 0, 256, wsgu16, 0, 8))
                jobs.append((wsu_d.rearrange("(p j) c -> p j c", j=8), 0, 256, wsgu16, 256, 8))
                wsd_v = wsd_d.rearrange("(p j) c -> p j c", j=2)
                for c0 in range(0, D, 512):
                    jobs.append((wsd_v, c0, 512, wsd16, c0, 2))
                for (src, c0, cw, dst, d0, nj) in jobs:
                    s_ = stg[k % 2]
                    q = dq[k % 2]
                    sc.dma(q, lambda s_=s_, src=src, c0=c0, cw=cw, nj=nj, q=q: sc.eng[q].dma_start(
                        out=s_[:, 0:nj, 0:cw], in_=src[:, :, c0:c0 + cw]), W=[s_])
                    e = "dve" if k % 2 == 0 else "act"
                    if e == "dve":
                        sc.op("dve", lambda s_=s_, dst=dst, d0=d0, cw=cw, nj=nj: V.tensor_copy(dst[:, 0:nj, d0:d0 + cw], s_[:, 0:nj, 0:cw]),
                              R=[s_], W=[dst], acc=True)
                    else:
                        sc.op("act", lambda s_=s_, dst=dst, d0=d0, cw=cw, nj=nj: A.copy(dst[:, 0:nj, d0:d0 + cw], s_[:, 0:nj, 0:cw]),
                              R=[s_], W=[dst], acc=True)
                    k += 1
                bst = stg[0][:].rearrange("p j c -> p (j c)")[:, 0:INW]
                sc.dma("sp", lambda: nc.sync.dma_start(out=bst, in_=bin_d.partition_broadcast(128)), W=[stg[0]])
                sc.op("dve", lambda: V.tensor_copy(binb[:], bst), R=[stg[0]], W=[binb])
                for (dst, src) in ((bgk2b, bgk2_d), (gnb, gnorm_d), (bog1, bo_d), (ln1g, ln1g_d), (ln1b, ln1b_d),
                                   (rbb, rb_d), (esink, sinks_d)):
                    sc.dma("sp", lambda dst=dst, src=src: nc.sync.dma_start(out=dst[:], in_=src.partition_broadcast(128)), W=[dst])
                sc.dma("sp", lambda: nc.sync.dma_start(out=wgk2[:], in_=wgk2_d[:, :]), W=[wgk2])
                sc.dma("sp", lambda: nc.sync.dma_start(out=bgkT[:], in_=bgkT_d[:, :]), W=[bgkT])
                sc.op("act", lambda: A.activation(esink[:], esink[:], AF.Exp), R=[esink], W=[esink])
                sc.op("dve", lambda: V.tensor_tensor(bog1[:], bog1[:], G1(), ALU.mult), R=[bog1, modv], W=[bog1])
                posi = sb(sl, "posi", [128, NT], I32); posf = sb(sl, "posf", [128, NT])
                ang = sb(sl, "ang", [128, NT, 32]); fr = sb(sl, "fr", [128, NT, 32]); ki = sb(sl, "ki", [128, NT, 32], I32)
                kf = sb(sl, "kf", [128, NT, 32]); m1 = sb(sl, "m1", [128, NT, 32])
                sc.dma("sp", lambda: nc.sync.dma_start(out=posi[:], in_=pos_d[:, :]), W=[posi])
                sc.op("dve", lambda: V.tensor_copy(posf[:], posi[:]), R=[posi], W=[posf])
                for i in range(NT):
                    sc.op("dve", lambda i=i: V.tensor_scalar(ang[:, i, :], cst[:, C_INVF:C_INVF + 32], posf[:, i:i + 1], None, ALU.mult),
                          R=[posf, cst], W=[ang], acc=(i > 0))
                for (tab, off) in ((sinT, 0.5), (cosT, 0.75)):
                    sc.op("dve", lambda off=off: V.tensor_scalar(fr[:], ang[:], 1.0 / (2 * np.pi), off, ALU.mult, ALU.add), R=[ang], W=[fr])
                    sc.op("dve", lambda: V.tensor_copy(ki[:], fr[:]), R=[fr], W=[ki])
                    sc.op("dve", lambda: V.tensor_copy(kf[:], ki[:]), R=[ki], W=[kf])
                    sc.op("dve", lambda: V.tensor_tensor(fr[:], fr[:], kf[:], ALU.subtract), R=[fr, kf], W=[fr])
                    sc.op("dve", lambda: V.tensor_scalar(m1[:], fr[:], 0.0, None, ALU.is_lt), R=[fr], W=[m1])
                    sc.op("dve", lambda: V.tensor_tensor(fr[:], fr[:], m1[:], ALU.add), R=[fr, m1], W=[fr])
                    sc.op("dve", lambda: V.tensor_scalar(m1[:], fr[:], 1.0, None, ALU.is_ge), R=[fr], W=[m1])
                    sc.op("dve", lambda: V.tensor_tensor(fr[:], fr[:], m1[:], ALU.subtract), R=[fr, m1], W=[fr])
                    sc.op("dve", lambda: V.tensor_scalar(fr[:], fr[:], 2 * np.pi, -np.pi, ALU.mult, ALU.add), R=[fr], W=[fr])
                    sc.op("dve", lambda: V.tensor_scalar(fr[:], fr[:], 3.141592, -3.141592, ALU.min, ALU.max), R=[fr], W=[fr])
                    sc.op("act", lambda tab=tab: A.activation(tab[:], fr[:], AF.Sin), R=[fr], W=[tab])

                sc1T = sb(sl, "sc1T", [128, 8]); sh1T = sb(sl, "sh1T", [128, 8]); shb = sb(sl, "shb", [128, 8, 128], BF16)
                for (dstT, sec) in ((sc1T, 1), (sh1T, 0)):
                    for j in range(8):
                        sc.op("pe", lambda j=j, sec=sec: PE.transpose(pm[0][:, 0:128], modv[:, sec * D + j * 128:sec * D + (j + 1) * 128], ident()), R=[modv, cst], W=[pm[0]])
                        sc.op("act", lambda j=j, dstT=dstT: A.copy(dstT[:, j:j + 1], pm[0][:, 0:1]), R=[pm[0]], W=[Acc(dstT)] if j > 0 else [dstT])
                for j in range(8):
                    sc.op("dve", lambda j=j: V.tensor_scalar(shb[:, j, :], cst[:, C_ON:C_ON + 128], sh1T[:, j:j + 1], None, ALU.mult), R=[sh1T, cst], W=[Acc(shb)] if j > 0 else [shb])
                gi = 0
                for c0 in range(0, INW, 512):
                    cw = min(512, INW - c0)
                    p = pm[1 + gi % 2]
                    for j in range(8):
                        sc.op("pe", lambda j=j, p=p, c0=c0, cw=cw: PE.matmul(p[:, 0:cw], shb[:, j, :], win16[:, j, c0:c0 + cw], start=(j == 0), stop=(j == 7)),
                              R=[shb, win16], W=[p], acc=(j > 0))
                    sc.op("dve", lambda p=p, c0=c0, cw=cw: V.tensor_tensor(binb[:, c0:c0 + cw], p[:, 0:cw], binb[:, c0:c0 + cw], ALU.add), R=[p, binb], W=[binb])
                    gi += 1
                gkb = sb(sl, "gkb", [128, 128])
                sc.op("dve", lambda: V.memset(gkb[:], 0.0), W=[gkb])
                sc.op("dve", lambda: V.tensor_copy(gkb[:, 0:16], binb[:, 2304:2320]), R=[binb], W=[gkb])
                sc.op("pe", lambda: PE.transpose(pm[0][:, 0:128], gkb[:], ident()), R=[gkb, cst], W=[pm[0]])
                sc.op("act", lambda: A.copy(bgkT[:], pm[0][0:16, 0:1]), R=[pm[0]], W=[bgkT])
                for j in range(8):
                    sc.op("dve", lambda j=j: V.tensor_scalar(win16[:, j, :], win16[:, j, :], sc1T[:, j:j + 1], None, ALU.mult), R=[win16, sc1T], W=[win16])
                    sc.op("dve", lambda j=j: V.tensor_tensor(wo16[:, j, :], wo16[:, j, :], G1(), ALU.mult), R=[wo16, modv], W=[wo16])
                for j in range(2):
                    sc.op("dve", lambda j=j: V.tensor_tensor(wsd16[:, j, :], wsd16[:, j, :], G2(), ALU.mult), R=[wsd16, modv], W=[wsd16])
            sc.barrier()
            xt = sb(sa, "xt", [128, D]); t32a = sb(sa, "t32a", [128, D]); t32b = sb(sa, "t32b", [128, D])
            stats = sb(sa, "stats", [128, 12]); mv = sb(sa, "mv", [128, 2]); rstd = sb(sa, "rstd", [128, 1])
            stats1 = sb(sa, "stats1", [128, 12]); mv1 = sb(sa, "mv1", [128, 2]); rstd1 = sb(sa, "rstd1", [128, 1]); nmr1 = sb(sa, "nmr1", [128, 1]); nmr = sb(sa, "nmr", [128, 1])
            h16 = sb(sa, "h16", [128, D], BF16); hT = sb(sa, "hT", [128, 8, 128], BF16)
            projs = [sb(sa, "proj", [128, INW], BF16), Buf(modv.t[:, 0:1160].bitcast(BF16))]
            sglb = Buf(modv.t[:, 1160:1800])
            qkr = sb(sa, "qkr", [128, 640], BF16)
            qT = sb(sa, "qT", [128, 512], BF16)
            kT = [sb(sa, "kT%d" % i, [128, 128], BF16) for i in range(2)]
            vext = [sb(sa, "vext%d" % i, [128, 2, 65], BF16) for i in range(2)]
            ecur = sb(sa, "ecur", [128, 512], BF16); eprev = sb(sa, "eprev", [128, 512], BF16)
            mp4 = sb(sa, "mp4", [128, 512], BF16); mc4 = sb(sa, "mc4", [128, 512], BF16); tri4 = sb(sa, "tri4", [128, 512], BF16)
            den = sb(sa, "den", [128, 8])
            mixeds = [sb(sa, "mixed%d" % i_, [128, D], BF16) for i_ in range(3)]; resids = [sb(sa, "resid%d" % i_, [128, D], BF16) for i_ in range(3)]; mixT = sb(sa, "mixT", [128, 8, 128], BF16)
            gkTs = [sb(sa, "gkT%d" % i_, [16, 128]) for i_ in range(2)]; zg = sb(sa, "zg", [128, 256]); lg = sb(sa, "lg", [128, 256])
            bsb = sb(sa, "bsb", [128, 256]); eb = sb(sa, "eb", [128, 256]); enb = sb(sa, "enb", [128, 256]); ebl = sb(sa, "ebl", [128, 256])
            qin = sb(sa, "qin", [128, 320], BF16); kin = sb(sa, "kin", [128, 320], BF16); kout = sb(sa, "kout", [128, 256], BF16)
            vl16 = sb(sa, "vl16", [128, 512], BF16)
            qkT = sb(sa, "qkT", [128, 8, 128], BF16)
            aT = sb(sa, "aT", [128, 512], BF16)
            dec = sb(sa, "dec", [128, 8])
            S32 = sb(sa, "S32", [128, 4, 128]); S16a = sb(sa, "S16a", [128, 4, 128], BF16); S16b = sb(sa, "S16b", [128, 4, 128], BF16)
            blx = sb(sa, "blx", [128, 320])
            qTA = sb(sa, "qTA", [128, 4, 128], BF16); qTB = sb(sa, "qTB", [128, 4, 128], BF16)
            ssq = sb(sa, "ssq", [128, 4]); sgl = sglb
            h2p = sb(sa, "h2p", [128, D], BF16); h2T = sb(sa, "h2T", [128, 8, 128], BF16)
            scores, biased, masked, sel, gw, posfull = [sb(sa, "rs%d" % i_, [128, NE]) for i_ in range(6)]
            oneh = sb(sa, "oneh", [128, NE])
            g8 = sb(sa, "g8", [128, 8, 8]); gs = sb(sa, "gs", [128, 8]); gtop = sb(sa, "gtop", [128, 8]); gmask = sb(sa, "gmask", [128, 8])
            pen = sb(sa, "pen", [128, 8]); top8 = sb(sa, "top8", [128, 8]); idx8 = sb(sa, "idx8", [128, 8], U32)
            sumw = sb(sa, "sumw", [128, 1])
            sg = oneh; actp = sb(sa, "actp", [128, 256], BF16); actT = sb(sa, "actT", [128, 2, 128], BF16)

            for r in range(4):
                sc.op("pool", lambda r=r: P.tensor_copy(mp4[:, r * 128:(r + 1) * 128], cb16[:, C_MP:C_MP + 128]), R=[cb16], W=[mp4], acc=True)
                sc.op("pool", lambda r=r: P.tensor_copy(mc4[:, r * 128:(r + 1) * 128], cb16[:, C_MC:C_MC + 128]), R=[cb16], W=[mc4], acc=True)
                sc.op("pool", lambda r=r: P.tensor_copy(tri4[:, r * 128:(r + 1) * 128], cb16[:, C_TRI:C_TRI + 128]), R=[cb16], W=[tri4], acc=True)
            sc.op("pool", lambda: P.memset(S32[:], 0.0), W=[S32])
            sc.op("pool", lambda: P.memset(qTA[:], 0.0), W=[qTA])
            sc.op("pool", lambda: P.memset(blx[:], 0.0), W=[blx])
            sc.op("pool", lambda: P.memset(qin[:], 0.0), W=[qin])
            sc.op("pool", lambda: P.memset(kin[:], 0.0), W=[kin])
            sc.op("pool", lambda: P.memset(qTB[:], 0.0), W=[qTB])
            sc.op("pool", lambda: P.memset(S16a[:], 0.0), W=[S16a])
            for b_ in vext:
                sc.op("pool", lambda b_=b_: P.memset(b_[:], 1.0), W=[b_])

            psA = Buf(ps_tr.t[:, 0:512]); psB = Buf(ps_tr.t[:, 512:1024])

            def transposes(src, n, dst_ap, dstbuf, pst=None):
                pst = ps_tr if pst is None else pst
                for j in range(n):
                    sc.op("pe", lambda j=j: PE.transpose(pst[:, j * 128:(j + 1) * 128], src[:, j * 128:(j + 1) * 128], id16()),
                          R=[src, cb16], W=[pst], acc=(j > 0))
                sc.op("act", lambda: A.copy(dst_ap, pst[:, 0:n * 128]), R=[pst], W=[dstbuf])

            ntile = int(os.environ.get("K_NTILE", NT))
            kstop = float(os.environ.get("K_STOP", 99))
            def seg_a(i):
                cur, prv = i % 2, (i + 1) % 2
                r0 = i * 128
                mixed = mixeds[i % 3]; resid = resids[i % 3]; proj = projs[i % 2]; gkT = gkTs[i % 2]
                sc.dma("sp", lambda: nc.sync.dma_start(out=xt[:], in_=x_d[r0:r0 + 128, :]), W=[xt])
                ln_stats((stats1, mv1, rstd1, nmr1), xt, [xt])
                sc.op("dve", lambda: V.scalar_tensor_tensor(resid[:], xt[:], ALPHA, bog1[:], ALU.mult, ALU.add), R=[xt, bog1], W=[resid])
                sc.op("act", lambda: A.activation(h16[:], xt[:], AF.Identity, bias=nmr1[:, 0:1], scale=rstd1[:, 0:1]), R=[xt, nmr1, rstd1], W=[h16])
                for rr_ in range(2):
                    for j in range(4):
                        jj = rr_ * 4 + j
                        sc.op("pe", lambda j=j, jj=jj: PE.transpose(psA[:, j * 128:(j + 1) * 128], h16[:, jj * 128:(jj + 1) * 128], id16()), R=[h16, cb16], W=[psA], acc=(j > 0))
                    sc.op("act", lambda rr_=rr_: A.copy(hT[:, rr_ * 4:rr_ * 4 + 4, :].rearrange("p j t -> p (j t)"), psA[:, 0:512]), R=[psA], W=[Acc(hT)] if rr_ else [hT])

            def seg_b(i):
                cur, prv = i % 2, (i + 1) % 2
                r0 = i * 128
                mixed = mixeds[i % 3]; resid = resids[i % 3]; proj = projs[i % 2]; gkT = gkTs[i % 2]
                gi = 0
                for c0 in range(0, INW, 512):
                    cw = min(512, INW - c0)
                    p = pm[gi % 2]
                    for j in range(8):
                        sc.op("pe", lambda j=j, p=p, c0=c0, cw=cw: PE.matmul(p[:, 0:cw], hT[:, j, :], win16[:, j, c0:c0 + cw], start=(j == 0), stop=(j == 7)),
                              R=[hT, win16], W=[p], acc=(j > 0))
                    sc.op("dve", lambda p=p, c0=c0, cw=cw: V.tensor_tensor(proj[:, c0:c0 + cw], p[:, 0:cw], binb[:, c0:c0 + cw], ALU.add),
                          R=[p, binb], W=[proj], acc=True)
                    gi += 1
                for j in range(8):
                    sc.op("pe", lambda j=j: PE.matmul(pm[1][0:16, 0:128], win16[:, j, 2304:2320], hT[:, j, :], start=(j == 0), stop=(j == 7)),
                          R=[hT, win16], W=[pm[1]], acc=(j > 0))
                sc.op("dve", lambda: V.tensor_scalar(gkT[:], pm[1][0:16, 0:128], bgkT[:, 0:1], None, ALU.add), R=[pm[1], bgkT], W=[gkT])

            def seg_c(i):
                cur, prv = i % 2, (i + 1) % 2
                r0 = i * 128
                mixed = mixeds[i % 3]; resid = resids[i % 3]; proj = projs[i % 2]; gkT = gkTs[i % 2]
                qk = proj[:, 0:640].rearrange("p (h two d) -> p h two d", two=2, d=32)
                qo = qkr[:].rearrange("p (h two d) -> p h two d", two=2, d=32)
                cB = cosT[:, i, :].unsqueeze(1).broadcast_to([128, 10, 32])
                sB = sinT[:, i, :].unsqueeze(1).broadcast_to([128, 10, 32])
                sc.op("dve", lambda: V.tensor_tensor(xt[:, 0:320].rearrange("p (h d) -> p h d", d=32), qk[:, :, 0, :], cB, ALU.mult), R=[proj, cosT], W=[xt])
                sc.op("pool", lambda: P.tensor_tensor(xt[:, 320:640].rearrange("p (h d) -> p h d", d=32), qk[:, :, 1, :], sB, ALU.mult), R=[proj, sinT], W=[xt])
                sc.op("dve", lambda: V.tensor_tensor(qo[:, :, 0, :], xt[:, 0:320].rearrange("p (h d) -> p h d", d=32), xt[:, 320:640].rearrange("p (h d) -> p h d", d=32), ALU.subtract), R=[xt], W=[qkr])
                sc.op("dve", lambda: V.tensor_tensor(xt[:, 0:320].rearrange("p (h d) -> p h d", d=32), qk[:, :, 1, :], cB, ALU.mult), R=[proj, cosT], W=[xt])
                sc.op("pool", lambda: P.tensor_tensor(xt[:, 320:640].rearrange("p (h d) -> p h d", d=32), qk[:, :, 0, :], sB, ALU.mult), R=[proj, sinT], W=[xt])
                sc.op("dve", lambda: V.tensor_tensor(qo[:, :, 1, :], xt[:, 0:320].rearrange("p (h d) -> p h d", d=32), xt[:, 320:640].rearrange("p (h d) -> p h d", d=32), ALU.add), R=[xt], W=[qkr], acc=True)
                for j in range(4):
                    sc.op("pe", lambda j=j: PE.transpose(psA[:, j * 128:(j + 1) * 128], qkr[:, j * 128:(j + 1) * 128], id16()), R=[qkr, cb16], W=[psA], acc=(j > 0))
                sc.op("act", lambda: A.copy(qT[:], psA[:, 0:512]), R=[psA], W=[qT])
                sc.op("pe", lambda: PE.transpose(psA[:, 0:128], qkr[:, 512:640], id16()), R=[qkr, cb16], W=[psA])
                sc.op("act", lambda: A.copy(kT[cur][:], psA[:, 0:128]), R=[psA], W=[kT[cur]])
                sc.op("pool", lambda: P.tensor_copy(vext[cur][:, :, 0:64], proj[:, 640:768].rearrange("p (g d) -> p g d", d=64)),
                      R=[proj], W=[vext[cur]])

            def seg_d(i):
                cur, prv = i % 2, (i + 1) % 2
                r0 = i * 128
                mixed = mixeds[i % 3]; resid = resids[i % 3]; proj = projs[i % 2]; gkT = gkTs[i % 2]
                for g in range(2):
                    pr = slice(g * 64, (g + 1) * 64)
                    sc.op("pe", lambda: PE.matmul(pm[0][:], kT[cur][pr, :], qT[pr, :], start=True, stop=True), R=[kT[cur], qT], W=[pm[0]])
                    sc.op("act", lambda: A.activation(ecur[:], pm[0][:], AF.Exp, scale=0.125), R=[pm[0]], W=[ecur])
                    sc.op("pool", lambda: P.tensor_tensor(ecur[:], ecur[:], mc4[:], ALU.mult), R=[ecur, mc4], W=[ecur])
                    if i > 0:
                        sc.op("pe", lambda: PE.matmul(pm[0][:], kT[prv][pr, :], qT[pr, :], start=True, stop=True), R=[kT[prv], qT], W=[pm[0]])
                        sc.op("act", lambda: A.activation(eprev[:], pm[0][:], AF.Exp, scale=0.125), R=[pm[0]], W=[eprev])
                        sc.op("pool", lambda: P.tensor_tensor(eprev[:], eprev[:], mp4[:], ALU.mult), R=[eprev, mp4], W=[eprev])
                    po = pm[1]
                    for c in range(4):
                        oc = slice(c * 65, (c + 1) * 65)
                        if i > 0:
                            sc.op("pe", lambda c=c, oc=oc: PE.matmul(po[:, oc], eprev[:, c * 128:(c + 1) * 128], vext[prv][:, g, :], start=True, stop=False),
                                  R=[eprev, vext[prv]], W=[po], acc=(c > 0))
                        sc.op("pe", lambda c=c, oc=oc: PE.matmul(po[:, oc], ecur[:, c * 128:(c + 1) * 128], vext[cur][:, g, :], start=(i == 0), stop=True),
                              R=[ecur, vext[cur]], W=[po], acc=(c > 0 or i > 0))
                    pov = po[:, 0:260].rearrange("p (c e) -> p c e", e=65)
                    sc.op("dve", lambda: V.tensor_tensor(den[:, g * 4:(g + 1) * 4], pov[:, :, 64], esink[:, g * 4:(g + 1) * 4], ALU.add),
                          R=[po, esink], W=[den])
                    sc.op("dve", lambda: V.reciprocal(den[:, g * 4:(g + 1) * 4], den[:, g * 4:(g + 1) * 4]), R=[den], W=[den])
                    for c in range(4):
                        h = g * 4 + c
                        sc.op("act", lambda c=c, h=h: A.activation(mixed[:, h * 64:(h + 1) * 64], po[:, c * 65:c * 65 + 64], AF.Copy, scale=den[:, h:h + 1]),
                              R=[po, den], W=[mixed], acc=True)

            def seg_e(i):
                cur, prv = i % 2, (i + 1) % 2
                r0 = i * 128
                mixed = mixeds[i % 3]; resid = resids[i % 3]; proj = projs[i % 2]; gkT = gkTs[i % 2]
                sc.op("pe", lambda: PE.matmul(pm[2][:, 0:256], gkT[:], wgk2[:], start=True, stop=True), R=[gkT, wgk2], W=[pm[2]])
                sc.op("dve", lambda: V.tensor_tensor(zg[:], pm[2][:, 0:256], bgk2b[:], ALU.add), R=[pm[2], bgk2b], W=[zg])
                sc.op("act", lambda: A.activation(zg[:], zg[:], AF.Exp, scale=-1.0), R=[zg], W=[zg])
                sc.op("act", lambda: A.activation(zg[:], zg[:], AF.Ln, bias=1.0, scale=1.0), R=[zg], W=[zg])
                sc.op("dve", lambda: V.tensor_scalar(lg[:], zg[:], -1.0 / 16.0, None, ALU.mult), R=[zg], W=[lg])
                sc.op("pe", lambda: PE.matmul(pm[3][:, 0:256], cst[:, C_TRI:C_TRI + 128], lg[:], start=True, stop=True), R=[cst, lg], W=[pm[3]])
                sc.op("pe", lambda: PE.matmul(pm[3][:, 256:512], cst[:, C_BO:C_BO + 128], lg[:], start=True, stop=True), R=[cst, lg], W=[pm[3]], acc=True)
                sc.op("act", lambda: A.copy(blx[:, 0:256], pm[3][:, 256:512]), R=[pm[3]], W=[blx])
                for hd in range(4):
                    sc.op("pe", lambda hd=hd: PE.transpose(pm[2][:, hd * 128:(hd + 1) * 128], blx[:, hd * 64:hd * 64 + 128], ident()),
                          R=[blx, cst], W=[pm[2]], acc=(hd > 0))
                pT = pm[2][0:64, :].rearrange("p (h t) -> p h t", t=128)
                for ch in range(2):
                    sc.op("act", lambda ch=ch: A.activation(dec[0:64, ch * 4:ch * 4 + 4], pT[:, :, ch * 64], AF.Exp), R=[pm[2]], W=[dec], acc=(ch > 0))
                sc.op("act", lambda: A.copy(bsb[:], pm[3][:, 0:256]), R=[pm[3]], W=[bsb])
                sc.op("act", lambda: A.activation(eb[:], bsb[:], AF.Exp), R=[bsb], W=[eb])
                sc.op("act", lambda: A.activation(enb[:], bsb[:], AF.Exp, scale=-1.0), R=[bsb], W=[enb])
                sc.op("dve", lambda: V.tensor_tensor(ebl[:], pm[3][:, 256:512], bsb[:], ALU.subtract), R=[pm[3], bsb], W=[ebl])
                sc.op("act", lambda: A.activation(ebl[:], ebl[:], AF.Exp), R=[ebl], W=[ebl])
                sc.op("dve", lambda: V.scalar_tensor_tensor(qin[:, 0:256], proj[:, 768:1024], 0.125, eb[:], ALU.mult, ALU.mult), R=[proj, eb], W=[qin])
                sc.op("pool", lambda: P.tensor_tensor(kin[:, 0:256], proj[:, 1024:1280], enb[:], ALU.mult), R=[proj, enb], W=[kin])
                sc.op("pool", lambda: P.tensor_tensor(kout[:], proj[:, 1024:1280], ebl[:], ALU.mult), R=[proj, ebl], W=[kout])
                sc.op("pool", lambda: P.tensor_copy(vl16[:], proj[:, 1280:1792]), R=[proj], W=[vl16])
                for hd in range(4):
                    sc.op("pe", lambda hd=hd: PE.transpose(psB[:, hd * 128:(hd + 1) * 128], qin[:, hd * 64:hd * 64 + 128], id16()), R=[qin, cb16], W=[psB], acc=(hd > 0))
                sc.op("act", lambda: A.copy(qkT[0:64, 0:4, :].rearrange("p j t -> p (j t)"), psB[0:64, 0:512]), R=[psB], W=[qkT])
                pq = psB[0:64, 0:512].rearrange("p (h t) -> p h t", t=128)
                sc.op("act", lambda: A.copy(qTA[0:64, :, 0:64], pq[:, :, 0:64]), R=[psB], W=[qTA])
                sc.op("act", lambda: A.copy(qTB[0:64, :, 64:128], pq[:, :, 64:128]), R=[psB], W=[qTB])
                for hd in range(4):
                    sc.op("pe", lambda hd=hd: PE.transpose(psB[:, hd * 128:(hd + 1) * 128], kin[:, hd * 64:hd * 64 + 128], id16()), R=[kin, cb16], W=[psB], acc=(hd > 0))
                sc.op("act", lambda: A.copy(qkT[0:64, 4:8, :].rearrange("p j t -> p (j t)"), psB[0:64, 0:512]), R=[psB], W=[Acc(qkT)])
                for hd in range(4):
                    sc.op("pe", lambda hd=hd: PE.matmul(pm[3][:, hd * 128:(hd + 1) * 128], qkT[0:64, 4 + hd, :], qkT[0:64, hd, :], start=True, stop=True),
                          R=[qkT], W=[pm[3]], acc=(hd > 0))
                sc.op("dve", lambda: V.tensor_tensor(aT[:], pm[3][:], tri4[:], ALU.mult), R=[pm[3], tri4], W=[aT])
                pu = pm[2]

                def u_and_state(ch, Sdst):
                    rr = slice(ch * 64, (ch + 1) * 64)
                    for hd in range(4):
                        sc.op("pe", lambda hd=hd: PE.matmul(pu[0:64, hd * 128:(hd + 1) * 128], kout[rr, hd * 64:(hd + 1) * 64],
                                                            vl16[rr, hd * 128:(hd + 1) * 128], start=True, stop=True),
                              R=[kout, vl16], W=[pu], acc=(hd > 0))
                    for hd in range(4):
                        sc.op("dve", lambda hd=hd: V.scalar_tensor_tensor(S32[0:64, hd, :], S32[0:64, hd, :], dec[0:64, ch * 4 + hd:ch * 4 + hd + 1],
                                                                          pu[0:64, hd * 128:(hd + 1) * 128], ALU.mult, ALU.add),
                              R=[S32, dec, pu], W=[S32], acc=(hd > 0))
                    sc.op("act", lambda: A.copy(Sdst[0:64], S32[0:64]), R=[S32], W=[Sdst])

                u_and_state(0, S16b)
                pg = pm[3]
                for hd in range(4):
                    oc = slice(hd * 128, (hd + 1) * 128)
                    sc.op("pe", lambda hd=hd, oc=oc: PE.matmul(pg[:, oc], aT[:, hd * 128:(hd + 1) * 128], vl16[:, oc], start=True, stop=False),
                          R=[aT, vl16], W=[pg], acc=(hd > 0))
                    sc.op("pe", lambda hd=hd, oc=oc: PE.matmul(pg[:, oc], qTA[0:64, hd, :], S16a[0:64, hd, :], start=False, stop=False),
                          R=[qTA, S16a], W=[pg], acc=True)
                    sc.op("pe", lambda hd=hd, oc=oc: PE.matmul(pg[:, oc], qTB[0:64, hd, :], S16b[0:64, hd, :], start=False, stop=True),
                          R=[qTB, S16b], W=[pg], acc=True)
                u_and_state(1, S16a)
                for hd in range(4):
                    sc.op("act", lambda hd=hd: A.activation(sglb[:, 512:640], pg[:, hd * 128:(hd + 1) * 128], AF.Square, accum_out=ssq[:, hd:hd + 1]),
                          R=[pg], W=[sglb, Acc(ssq)] if hd > 0 else [sglb, ssq])
                sc.op("act", lambda: A.activation(ssq[:], ssq[:], AF.Sqrt, bias=EPS, scale=1.0 / 128.0), R=[ssq], W=[ssq])
                sc.op("dve", lambda: V.reciprocal(ssq[:], ssq[:]), R=[ssq], W=[ssq])
                sc.op("act", lambda: A.activation(sgl[:, 0:512], proj[:, 1792:2304], AF.Silu), R=[proj], W=[sgl])
                sc.op("pool", lambda: P.tensor_tensor(sgl[:, 0:512], sgl[:, 0:512], gnb[:], ALU.mult), R=[sgl, gnb], W=[sgl])
                for hd in range(4):
                    sc.op("dve", lambda hd=hd: V.scalar_tensor_tensor(mixed[:, 512 + hd * 128:512 + (hd + 1) * 128], pg[:, hd * 128:(hd + 1) * 128], ssq[:, hd:hd + 1],
                                                                      sgl[:, hd * 128:(hd + 1) * 128], ALU.mult, ALU.mult),
                          R=[pg, ssq, sgl], W=[mixed], acc=True)
                if debug and i == 0:
                    sc.dma("sp", lambda: nc.sync.dma_start(out=dbg16_d[0, :, :], in_=mixed[:]), R=[mixed], W=[B_dbg], acc=True)
                    sc.dma("sp", lambda: nc.sync.dma_start(out=dbg16_d[1, :, 0:640], in_=qkr[:]), R=[qkr], W=[B_dbg], acc=True)
                    sc.dma("sp", lambda: nc.sync.dma_start(out=dbg16_d[2, :, 0:512], in_=ecur[:]), R=[ecur], W=[B_dbg], acc=True)
                    sc.dma("sp", lambda: nc.sync.dma_start(out=dbg16_d[2, :, 512:1024], in_=qT[:]), R=[qT], W=[B_dbg], acc=True)
                    sc.dma("sp", lambda: nc.sync.dma_start(out=dbg16_d[3, :, 0:128], in_=kT[cur][:]), R=[kT[cur]], W=[B_dbg], acc=True)
                    sc.dma("sp", lambda: nc.sync.dma_start(out=dbg16_d[3, :, 128:258], in_=vext[cur][:].rearrange("p g e -> p (g e)")), R=[vext[cur]], W=[B_dbg], acc=True)
                    sc.dma("sp", lambda: nc.sync.dma_start(out=dbg_d[3, :, 0:8], in_=den[:]), R=[den], W=[B_dbg], acc=True)
                    sc.dma("sp", lambda: nc.sync.dma_start(out=dbg_d[4, :, 0:256], in_=lg[:]), R=[lg], W=[B_dbg], acc=True)
                    sc.dma("sp", lambda: nc.sync.dma_start(out=dbg_d[5, :, 0:256], in_=bsb[:]), R=[bsb], W=[B_dbg], acc=True)
                    sc.dma("sp", lambda: nc.sync.dma_start(out=dbg_d[6, :, 0:8], in_=dec[:]), R=[dec], W=[B_dbg], acc=True)
                    sc.dma("sp", lambda: nc.sync.dma_start(out=dbg_d[6, :, 8:12], in_=ssq[:]), R=[ssq], W=[B_dbg], acc=True)

            def seg_f(i):
                cur, prv = i % 2, (i + 1) % 2
                r0 = i * 128
                mixed = mixeds[i % 3]; resid = resids[i % 3]; proj = projs[i % 2]; gkT = gkTs[i % 2]
                transposes(mixed, 8, mixT[:].rearrange("p j t -> p (j t)"), mixT, pst=ps_tr2)
                for hf in range(2):
                    p = pm[4 + hf]
                    for j in range(8):
                        sc.op("pe", lambda j=j, p=p, hf=hf: PE.matmul(p[:], mixT[:, j, :], wo16[:, j, hf * 512:(hf + 1) * 512], start=(j == 0), stop=(j == 7)),
                              R=[mixT, wo16], W=[p], acc=(j > 0))
                    sc.op("dve", lambda p=p, hf=hf: V.tensor_tensor(t32a[:, hf * 512:(hf + 1) * 512], p[:], resid[:, hf * 512:(hf + 1) * 512], ALU.add),
                          R=[p, resid], W=[t32a], acc=(hf > 0))
                ln_stats((stats, mv, rstd, nmr), t32a, [t32a])
                sc.op("act", lambda: A.activation(t32a[:], t32a[:], AF.Identity, bias=nmr[:, 0:1], scale=rstd[:, 0:1]), R=[t32a, nmr, rstd], W=[t32a])
                sc.op("dve", lambda: V.tensor_tensor(t32a[:], t32a[:], ln1g[:], ALU.mult), R=[t32a, ln1g], W=[t32a])
                sc.op("dve", lambda: V.tensor_tensor(t32a[:], t32a[:], ln1b[:], ALU.add), R=[t32a, ln1b], W=[t32a])
                if debug and i == 0:
                    sc.dma("sp", lambda: nc.sync.dma_start(out=dbg_d[0, :, :], in_=t32a[:]), R=[t32a], W=[B_dbg], acc=True)
                ln_stats((stats, mv, rstd, nmr), t32a, [t32a])
                sc.op("act", lambda: A.activation(t32b[:], t32a[:], AF.Identity, bias=nmr[:, 0:1], scale=rstd[:, 0:1]), R=[t32a, nmr, rstd], W=[t32b])
                sc.op("dve", lambda: V.tensor_tensor(t32b[:], t32b[:], SC2(), ALU.mult), R=[t32b, modv], W=[t32b])
                sc.op("dve", lambda: V.tensor_tensor(t32b[:], t32b[:], SH2(), ALU.add), R=[t32b, modv], W=[t32b])
                sc.op("act", lambda: A.copy(h2p[:].rearrange("t (j p) -> t p j", p=128), t32b[:].rearrange("t (p j) -> t p j", j=8)), R=[t32b], W=[h2p])
                sc.dma("act", lambda: A.dma_start(out=h2_s[r0:r0 + 128, :], in_=h2p[:]), R=[h2p], W=[B_h2s], acc=True)
                transposes(h2p, 8, h2T[:].rearrange("p j t -> p (j t)"), h2T, pst=ps_tr2)

            def seg_g(i):
                cur, prv = i % 2, (i + 1) % 2
                r0 = i * 128
                mixed = mixeds[i % 3]; resid = resids[i % 3]; proj = projs[i % 2]; gkT = gkTs[i % 2]
                for j in range(8):
                    sc.op("pe", lambda j=j: PE.matmul(pm[4][:, 0:256], h2T[:, j, :], wr16[:, j, :], start=(j == 0), stop=(j == 7)), R=[h2T, wr16], W=[pm[4]], acc=(j > 0))
                for j in range(8):
                    sc.op("pe", lambda j=j: PE.matmul(pm[5][:], h2T[:, j, :], wsgu16[:, j, :], start=(j == 0), stop=(j == 7)), R=[h2T, wsgu16], W=[pm[5]], acc=(j > 0))
                sc.op("act", lambda: A.activation(scores[:], pm[4][:, 0:256], AF.Sigmoid), R=[pm[4]], W=[scores])
                sc.op("act", lambda: A.activation(sg[:], pm[5][:, 0:256], AF.Silu), R=[pm[5]], W=[sg])
                sc.op("dve", lambda: V.tensor_tensor(actp[:].rearrange("t (j p) -> t p j", p=128), sg[:].rearrange("t (p j) -> t p j", j=2),
                                                     pm[5][:, 256:512].rearrange("t (p j) -> t p j", j=2), ALU.mult), R=[sg, pm[5]], W=[actp])
                transposes(actp, 2, actT[:].rearrange("p j t -> p (j t)"), actT, pst=ps_tr2)
                for hf in range(2):
                    p = pm[4 + hf]
                    for j in range(2):
                        sc.op("pe", lambda j=j, p=p, hf=hf: PE.matmul(p[:], actT[:, j, :], wsd16[:, j, hf * 512:(hf + 1) * 512], start=(j == 0), stop=(j == 1)),
                              R=[actT, wsd16], W=[p], acc=(j > 0))
                    sc.op("dve", lambda p=p, hf=hf: V.scalar_tensor_tensor(t32b[:, hf * 512:(hf + 1) * 512], t32a[:, hf * 512:(hf + 1) * 512], ALPHA, p[:], ALU.mult, ALU.add),
                          R=[p, t32a], W=[t32b], acc=(hf > 0))
                sc.dma("sp", lambda: nc.sync.dma_start(out=z_s[r0:r0 + 128, :], in_=t32b[:]), R=[t32b], W=[B_z], acc=True)

            def seg_h(i):
                cur, prv = i % 2, (i + 1) % 2
                r0 = i * 128
                mixed = mixeds[i % 3]; resid = resids[i % 3]; proj = projs[i % 2]; gkT = gkTs[i % 2]
                sc.op("dve", lambda: V.tensor_tensor(biased[:], scores[:], rbb[:], ALU.add), R=[scores, rbb], W=[biased])
                for g in range(8):
                    sc.op("dve", lambda g=g: V.max(out=g8[:, g, :], in_=biased[:, g * 32:(g + 1) * 32]), R=[biased], W=[g8], acc=(g > 0))
                sc.op("dve", lambda: V.tensor_tensor(gs[:], g8[:, :, 0], g8[:, :, 1], ALU.add), R=[g8], W=[gs])
                sc.op("dve", lambda: V.max(out=gtop[:], in_=gs[:]), R=[gs], W=[gtop])
                sc.op("dve", lambda: V.tensor_scalar(gmask[:], gs[:], gtop[:, 3:4], None, ALU.is_ge), R=[gs, gtop], W=[gmask])
                sc.op("dve", lambda: V.tensor_scalar(pen[:], gmask[:], BIG, -BIG, ALU.mult, ALU.add), R=[gmask], W=[pen])
                b3 = biased[:].rearrange("p (g e) -> p g e", e=32)
                m3 = masked[:].rearrange("p (g e) -> p g e", e=32)
                sc.op("dve", lambda: V.tensor_tensor(m3, b3, gmask[:].unsqueeze(2).broadcast_to([128, 8, 32]), ALU.mult), R=[biased, gmask], W=[masked])
                sc.op("dve", lambda: V.tensor_tensor(m3, m3, pen[:].unsqueeze(2).broadcast_to([128, 8, 32]), ALU.add), R=[masked, pen], W=[masked])
                sc.op("dve", lambda: V.max(out=top8[:], in_=masked[:]), R=[masked], W=[top8])
                sc.op("dve", lambda: V.tensor_scalar(sel[:], masked[:], top8[:, 7:8], None, ALU.is_ge), R=[masked, top8], W=[sel])
                sc.op("dve", lambda: V.scalar_tensor_tensor(gw[:], sel[:], 1.0, scores[:], ALU.mult, ALU.mult, accum_out=sumw[:]), R=[sel, scores], W=[gw, sumw])
                sc.op("dve", lambda: V.reciprocal(sumw[:], sumw[:]), R=[sumw], W=[sumw])
                sc.op("dve", lambda: V.tensor_scalar(gw[:], gw[:], sumw[:, 0:1], 2.5, ALU.mult, ALU.mult), R=[gw, sumw], W=[gw])
                sc.op("dve", lambda: V.max(out=w_t[:, i, :], in_=gw[:]), R=[gw], W=[Acc(w_t)])
                sc.op("dve", lambda: V.max_index(out=idx8[:], in_max=w_t[:, i, :], in_values=gw[:]), R=[w_t, gw], W=[idx8])
                sc.op("dve", lambda: V.tensor_copy(idxf_t[:, i, :], idx8[:]), R=[idx8], W=[Acc(idxf_t)])
                sc.op("pe", lambda: PE.matmul(pm[4][:, 0:256], cst[:, C_LS:C_LS + 128], sel[:], start=True, stop=True), R=[cst, sel], W=[pm[4]])
                sc.op("pe", lambda: PE.matmul(pm[4][:, 256:512], cst[:, C_ON:C_ON + 128], sel[:], start=True, stop=True), R=[cst, sel], W=[pm[4]], acc=True)
                sc.op("dve", lambda: V.tensor_tensor(posfull[:], pm[4][:, 0:256], cntb[:], ALU.add), R=[pm[4], cntb], W=[posfull])
                sc.op("dve", lambda: V.tensor_tensor(cntb[:], pm[4][:, 256:512], cntb[:], ALU.add), R=[pm[4], cntb], W=[cntb])
                sc.dma("sp", lambda: nc.sync.dma_start(out=pf_s[r0:r0 + 128, :], in_=posfull[:]), R=[posfull], W=[B_pf], acc=True)

            def P1(i):
                seg_a(i); seg_b(i); seg_c(i); seg_d(i)

            def P3(i):
                seg_f(i); seg_g(i); seg_h(i)

            for step in range(ntile + 2):
                fs = []
                if step < ntile:
                    fs.append(lambda step=step: P1(step))
                if 0 <= step - 1 < ntile:
                    fs.append(lambda step=step: seg_e(step - 1))
                if 0 <= step - 2 < ntile:
                    fs.append(lambda step=step: P3(step - 2))
                if len(fs) == 1:
                    fs[0]()
                else:
                    sc.interleave(*fs)
        sc.barrier()
        if debug and not os.environ.get('K_FULL'):
            sc.finish([B_dbg, B_z, B_h2s])
            return nc
        IOA = bass.IndirectOffsetOnAxis
        slot_t = sb(st, "slot_t", [128, NT, 8], I32)
        idxw = sb(st, "idxw", [128, NBLK], I32)
        nblk = int(os.environ.get("K_NBLK", NBLK))
        with ExitStack() as sbk:
            cnti = sb(sbk, "cnti", [128, NE], I32); padf = sb(sbk, "padf", [128, NE]); pend = sb(sbk, "pend", [128, NE])
            pstart = sb(sbk, "pstart", [128, NE]); ones256 = sb(sbk, "ones256", [128, NE]); jkb = sb(sbk, "jkb", [128, NE])
            onehb = sb(sbk, "onehb", [128, NE])
            blk = sb(sbk, "blk", [128, 4]); D4 = sb(sbk, "D4", [128, 4, 128]); tmpf = sb(sbk, "tmpf", [128, NBLK])
            psk = sb(sbk, "psk", [128, NT, 8]); sltf = sb(sbk, "sltf", [128, NT, 8])
            h2b = [sb(sbk, "h2b%d" % i, [128, D], BF16) for i in range(2)]
            sc.op("pool", lambda: P.memset(ones256[:], 1.0), W=[ones256])
            sc.op("dve", lambda: V.tensor_copy(cnti[:], cntb[:]), R=[cntb], W=[cnti])
            sc.op("dve", lambda: V.tensor_scalar(cnti[:], cnti[:], 127, None, ALU.add), R=[cnti], W=[cnti])
            sc.op("dve", lambda: V.tensor_scalar(cnti[:], cnti[:], 7, None, ALU.arith_shift_right), R=[cnti], W=[cnti])
            sc.op("dve", lambda: V.tensor_scalar(cnti[:], cnti[:], 7, None, ALU.logical_shift_left), R=[cnti], W=[cnti])
            sc.op("dve", lambda: V.tensor_copy(padf[:], cnti[:]), R=[cnti], W=[padf])
            sc.op("dve", lambda: V.tensor_tensor_scan(pend[:], ones256[:], padf[:], 0.0, ALU.mult, ALU.add), R=[ones256, padf], W=[pend])
            sc.op("dve", lambda: V.tensor_tensor(pstart[:], pend[:], padf[:], ALU.subtract), R=[pend, padf], W=[pstart])
            for j in range(4):
                sc.op("dve", lambda j=j: V.tensor_scalar(jkb[:], pend[:], cst[:, C_BV + j:C_BV + j + 1], 0.0, ALU.is_le, ALU.add, accum_out=blk[:, j:j + 1]),
                      R=[pend, cst], W=[jkb, Acc(blk)] if j > 0 else [jkb, blk])
            sc.op("dve", lambda: V.tensor_scalar(blk[:], blk[:], 255.0, None, ALU.min), R=[blk], W=[blk])
            for j in range(4):
                sc.op("dve", lambda j=j: V.tensor_scalar(D4[:, j, :], ident(), blk[:, j:j + 1], None, ALU.mult), R=[cst, blk], W=[D4], acc=(j > 0))
            sc.op("pe", lambda: PE.matmul(pm[0][:], cst[:, C_ON:C_ON + 128], D4[:].rearrange("p j t -> p (j t)"), start=True, stop=True), R=[cst, D4], W=[pm[0]])
            bef = sb(sbk, "bef", [128, NBLK]); chg = sb(sbk, "chg", [128, NBLK])
            sc.op("act", lambda: A.copy(bef[:], pm[0][:]), R=[pm[0]], W=[bef])
            sc.op("pool", lambda: P.memset(chg[:], 0.0), W=[chg])
            sc.op("dve", lambda: V.tensor_tensor(chg[:, 1:NBLK], bef[:, 1:NBLK], bef[:, 0:NBLK - 1], ALU.is_equal), R=[bef, chg], W=[chg])
            for qq in range(NSUB):
                sc.op("dve", lambda qq=qq: V.memset(chg[:, qq * (NBLK // NSUB):qq * (NBLK // NSUB) + 1], 0.0), R=[chg], W=[chg])
            sc.op("dve", lambda: V.tensor_scalar(tmpf[:], bef[:], 128.0, cst[:, C_PI:C_PI + 1], ALU.mult, ALU.add), R=[bef, cst], W=[tmpf])
            sc.op("dve", lambda: V.scalar_tensor_tensor(tmpf[:], chg[:], 16777216.0, tmpf[:], ALU.mult, ALU.add), R=[chg, tmpf], W=[tmpf])
            sc.op("dve", lambda: V.tensor_copy(idxw[:], tmpf[:]), R=[tmpf], W=[idxw])
            pfb = [sb(sbk, "pfb%d" % i_, [128, NE]) for i_ in range(2)]
            for i in range(ntile):
                pf = pfb[i % 2]
                sc.dma("act", lambda: A.dma_start(out=pf[:], in_=pf_s[i * 128:(i + 1) * 128, :]), R=[B_pf], W=[pf])
                sc.op("dve", lambda: V.tensor_tensor(pf[:], pf[:], pstart[:], ALU.add), R=[pf, pstart], W=[pf])
                for k in range(8):
                    sc.op("dve", lambda k=k: V.tensor_scalar(onehb[:], cst[:, C_IOTA:C_IOTA + 256], idxf_t[:, i, k:k + 1], None, ALU.is_equal), R=[cst, idxf_t], W=[onehb])
                    sc.op("dve", lambda k=k: V.scalar_tensor_tensor(jkb[:], onehb[:], 1.0, pf[:], ALU.mult, ALU.mult, accum_out=psk[:, i, k:k + 1]),
                          R=[onehb, pf], W=[jkb, Acc(psk)])
                sc.op("dve", lambda: V.tensor_copy(slot_t[:, i, :], psk[:, i, :]), R=[psk], W=[Acc(slot_t)])
                hb = h2b[i % 2]
                sc.dma("sp", lambda: nc.sync.dma_start(out=hb[:], in_=h2_s[i * 128:(i + 1) * 128, :]), R=[B_h2s], W=[hb])
                for k in range(8):
                    sc.dma("pool", lambda k=k: P.indirect_dma_start(out=xdisp, out_offset=IOA(ap=slot_t[:, i, k:k + 1], axis=0), in_=hb[:], in_offset=None),
                           R=[hb, slot_t], W=[B_xd], acc=True)
        sc.barrier()
        st_ps.close()
        NS = NSUB
        with ExitStack() as scx:
            ptx = [ps(scx, "ptx%d" % i, [128, 1024], BF16) for i in range(2)]
            pta = ps(scx, "pta", [128, 1024], BF16)
            pgus = [ps(scx, "pgu%d" % i, [128, 512]) for i in range(2)]
            pds = [ps(scx, "pd%d" % i, [128, 512]) for i in range(2)]
            wg16 = [sb(scx, "wg16_%d" % i, [128, 2048], BF16) for i in range(NS)]
            wu16 = [sb(scx, "wu16_%d" % i, [128, 2048], BF16) for i in range(NS)]
            wd16 = [sb(scx, "wd16_%d" % i, [128, 2048], BF16) for i in range(NS)]
            xb = [sb(scx, "xb%d" % i, [128, D], BF16) for i in range(NS)]
            xT = [sb(scx, "xT%d" % i, [128, 8, 128], BF16) for i in range(2)]
            acT = [sb(scx, "acT%d" % i, [128, 2, 128], BF16) for i in range(2)]
            y32 = [sb(scx, "y32_%d" % i, [128, D], BF16) for i in range(2)]
            sgc = [sb(scx, "sgc%d" % i, [128, 256]) for i in range(2)]; acp = [sb(scx, "acp%d" % i, [128, 256], BF16) for i in range(2)]
            bof = lambda t: t // NS + (NBLK // NS) * (t % NS)
            bnd_reg = P.to_reg(NE * 128 - 1)

            def loads(t):
                p = t % NS
                bb = bof(t)
                ix = idxw[:, bb:bb + 1]
                for (dst, src) in ((wg16[p], weg_d), (wu16[p], weu_d), (wd16[p], wed_d)):
                    sc.dma("pool", lambda dst=dst, src=src: P.indirect_dma_start(out=dst[:], out_offset=None, in_=src, in_offset=IOA(ap=ix, axis=0),
                                                                                 bounds_check=bnd_reg, oob_is_err=False), R=[idxw], W=[dst])
                sc.dma("sp", lambda: nc.sync.dma_start(out=xb[p][:], in_=xdisp[bb * 128:(bb + 1) * 128, :]), R=[B_xd], W=[xb[p]])

            def fa(t):
                p, q = t % NS, t % 2
                for j in range(8):
                    sc.op("pe", lambda j=j: PE.transpose(ptx[q][:, j * 128:(j + 1) * 128], xb[p][:, j * 128:(j + 1) * 128], id16()), R=[xb[p], cb16], W=[ptx[q]], acc=(j > 0))
                sc.op("act", lambda: A.copy(xT[q][:].rearrange("p j t -> p (j t)"), ptx[q][:, :]), R=[ptx[q]], W=[xT[q]])

            def fb(t):
                p, q = t % NS, t % 2
                pgu = pgus[q]
                for (w16, c0) in ((wg16[p], 0), (wu16[p], 256)):
                    for j in range(8):
                        sc.op("pe", lambda j=j, w16=w16, c0=c0: PE.matmul(pgu[:, c0:c0 + 256], xT[q][:, j, :], w16[:, j * 256:(j + 1) * 256], start=(j == 0), stop=(j == 7)),
                              R=[xT[q], w16], W=[pgu], acc=(j > 0 or c0 > 0))
                sc.op("act", lambda: A.activation(sgc[q][:], pgu[:, 0:256], AF.Silu), R=[pgu], W=[sgc[q]])
                sc.op("dve", lambda: V.tensor_tensor(acp[q][:].rearrange("t (j p) -> t p j", p=128), sgc[q][:].rearrange("t (p j) -> t p j", j=2),
                                                     pgu[:, 256:512].rearrange("t (p j) -> t p j", j=2), ALU.mult), R=[sgc[q], pgu], W=[acp[q]])

            def fc(t):
                q = t % 2
                for j in range(2):
                    sc.op("pe", lambda j=j: PE.transpose(pta[:, j * 128:(j + 1) * 128], acp[q][:, j * 128:(j + 1) * 128], id16()), R=[acp[q], cb16], W=[pta], acc=(j > 0))
                sc.op("act", lambda: A.copy(acT[q][:].rearrange("p j t -> p (j t)"), pta[:, 0:256]), R=[pta], W=[acT[q]])

            def back(t):
                p, q = t % NS, t % 2
                bb = bof(t)
                for hf in range(2):
                    pp = pds[hf]
                    for j in range(2):
                        sc.op("pe", lambda j=j, pp=pp, hf=hf: PE.matmul(pp[:], acT[q][:, j, :], wd16[p][:, j * 1024 + hf * 512:j * 1024 + (hf + 1) * 512], start=(j == 0), stop=(j == 1)),
                              R=[acT[q], wd16[p]], W=[pp], acc=(j > 0))
                sc.op("act", lambda: A.copy(y32[q][:, 0:512], pds[0][:]), R=[pds[0]], W=[y32[q]])
                sc.op("dve", lambda: V.tensor_copy(y32[q][:, 512:1024], pds[1][:]), R=[pds[1]], W=[y32[q]], acc=True)
                sc.dma("act", lambda: A.dma_start(out=ydisp[bb * 128:(bb + 1) * 128, :], in_=y32[q][:]), R=[y32[q]], W=[B_yd], acc=True)

            for t in range(min(NS - 1, nblk)):
                loads(t)
            fa(0)
            for t in range(nblk):
                fb(t)
                if t + 1 < nblk:
                    fa(t + 1)
                if t >= 1:
                    back(t - 1)
                fc(t)
                if t + NS - 1 < nblk:
                    loads(t + NS - 1)
            back(nblk - 1)
        sc.barrier()
        with ExitStack() as sd:
            ln2g = sb(sd, "ln2g", [128, D]); ln2b = sb(sd, "ln2b", [128, D])
            zts = [sb(sd, "zt%d" % i, [128, D]) for i in range(2)]; accs = [sb(sd, "accb%d" % i, [128, D]) for i in range(2)]
            yg = [sb(sd, "yg%d" % i, [128, D], BF16) for i in range(16)]
            pacc = [[ps(sd, "pacc%d_%d" % (a_, h_), [128, 512]) for h_ in range(2)] for a_ in range(2)]
            dg = [sb(sd, "dg%d" % i_, [128, 8, 128], BF16) for i_ in range(2)]
            stats = sb(sd, "stats2", [128, 12]); mv = sb(sd, "mv2", [128, 2]); rstd = sb(sd, "rstd2", [128, 1]); nmr2 = sb(sd, "nmr2", [128, 1])
            sc.dma("sp", lambda: nc.sync.dma_start(out=ln2g[:], in_=ln2g_d.partition_broadcast(128)), W=[ln2g])
            sc.dma("sp", lambda: nc.sync.dma_start(out=ln2b[:], in_=ln2b_d.partition_broadcast(128)), W=[ln2b])
            for i in range(ntile):
                zt, accb = zts[i % 2], accs[i % 2]
                sc.dma("sp", lambda: nc.sync.dma_start(out=zt[:], in_=z_s[i * 128:(i + 1) * 128, :]), R=[B_z], W=[zt])
                for k in range(8):
                    g_ = yg[(i % 2) * 8 + k]
                    sc.dma("pool", lambda: P.indirect_dma_start(out=g_[:], out_offset=None, in_=ydisp, in_offset=IOA(ap=slot_t[:, i, k:k + 1], axis=0)),
                           R=[B_yd, slot_t], W=[g_])
                dgi = dg[i % 2]
                for k in range(8):
                    sc.op("act", lambda k=k: A.activation(dgi[:, k, :], id16(), AF.Copy, scale=w_t[:, i, k:k + 1]), R=[cb16, w_t], W=[Acc(dgi)] if k > 0 else [dgi])
                for hf in range(2):
                    pa = pacc[i % 2][hf]
                    for k in range(8):
                        g_ = yg[(i % 2) * 8 + k]
                        sc.op("pe", lambda k=k, g_=g_, pa=pa, hf=hf: PE.matmul(pa[:], dgi[:, k, :], g_[:, hf * 512:(hf + 1) * 512], start=(k == 0), stop=(k == 7)),
                              R=[dgi, g_], W=[pa], acc=(k > 0))
                    sc.op("dve", lambda pa=pa, hf=hf: V.tensor_tensor(accb[:, hf * 512:(hf + 1) * 512], pa[:], modv[:, 5 * D + hf * 512:5 * D + (hf + 1) * 512], ALU.mult),
                          R=[pa, modv], W=[accb], acc=(hf > 0))
                sc.op("dve", lambda: V.tensor_tensor(accb[:], accb[:], zt[:], ALU.add), R=[accb, zt], W=[accb])
                ln_stats((stats, mv, rstd, nmr2), accb, [accb])
                sc.op("act", lambda: A.activation(accb[:], accb[:], AF.Identity, bias=nmr2[:, 0:1], scale=rstd[:, 0:1]), R=[accb, nmr2, rstd], W=[accb])
                sc.op("dve", lambda: V.tensor_tensor(accb[:], accb[:], ln2g[:], ALU.mult), R=[accb, ln2g], W=[accb])
                sc.op("dve", lambda: V.tensor_tensor(accb[:], accb[:], ln2b[:], ALU.add), R=[accb, ln2b], W=[accb])
                sc.dma("sp", lambda: nc.sync.dma_start(out=out_d[i * 128:(i + 1) * 128, :], in_=accb[:]), R=[accb], W=[B_out], acc=True)
        sc.finish([B_out])
    return nc


def make_consts():
    c = np.zeros((128, NCONST), np.float32)
    p = np.arange(128)
    c[:, C_ID:C_ID + 128] = np.eye(128)
    same = (p[:, None] // 64) == (p[None, :] // 64)
    c[:, C_TRI:C_TRI + 128] = (same & (p[:, None] <= p[None, :]))
    c[:, C_BO:C_BO + 128] = same
    c[:, C_LS:C_LS + 128] = (p[:, None] < p[None, :])
    c[:, C_ON:C_ON + 128] = 1.0
    c[:, C_MP:C_MP + 128] = (p[:, None] > p[None, :])
    c[:, C_MC:C_MC + 128] = (p[:, None] <= p[None, :])
    c[:, C_IOTA:C_IOTA + 256] = np.arange(256)[None, :]
    for j in range(4):
        c[:, C_BV + j] = 128.0 * (j * 128 + p)
    half = 32
    c[:, C_INVF:C_INVF + 32] = (10000.0 ** (-np.arange(half, dtype=np.float32) / half)).astype(np.float32)[None, :]
    c[:, C_PI] = p
    return c


QPERM = np.concatenate([np.arange(h * 64, (h + 1) * 64) for h in (0, 4, 1, 5, 2, 6, 3, 7)])


def make_in_maps(inputs, cores):
    f = lambda a: np.ascontiguousarray(a, dtype=np.float32)
    colperm = np.concatenate([QPERM, np.arange(512, INW)])
    w_in = f(inputs["w_in"][0][:, colperm])
    b_in = f(inputs["b_in"][0][colperm])
    shared = dict(
        consts=make_consts(), w_ada=f(inputs["w_ada"][0]), b_ada=f(inputs["b_ada"][0][None]), w_in=w_in, b_in=b_in[None],
        b_gklo=f(b_in[2304:2320].reshape(16, 1)), sinks=f(inputs["attn_sinks"][0][None]), w_gk2=f(inputs["w_gk2"][0]),
        b_gk2=f(inputs["b_gk2"][0][None]), gnorm=f(np.tile(inputs["gla_norm_g"][0], 4)[None]), w_o=f(inputs["w_o"][0]),
        b_o=f(inputs["b_o"][0][None]), ln1_g=f(inputs["ln1_g"][0][None]), ln1_b=f(inputs["ln1_b"][0][None]),
        w_router=f(inputs["w_router"][0]), router_bias=f(inputs["router_bias"][0][None]),
        w_exp_gate=f(inputs["w_exp_gate"][0]).reshape(-1, 2048), w_exp_up=f(inputs["w_exp_up"][0]).reshape(-1, 2048),
        w_exp_down=f(inputs["w_exp_down"][0]).reshape(-1, 2048),
        w_sh_gate=f(inputs["w_sh_gate"][0]), w_sh_up=f(inputs["w_sh_up"][0]), w_sh_down=f(inputs["w_sh_down"][0]),
        ln2_g=f(inputs["ln2_g"][0][None]), ln2_b=f(inputs["ln2_b"][0][None]),
    )
    maps = []
    for b in cores:
        m = dict(shared)
        m["x"] = f(inputs["x"][b])
        m["c"] = f(inputs["c"][b].reshape(128, 8))
        m["pos"] = np.ascontiguousarray(inputs["positions"][b].reshape(NT, 128).T.astype(np.int32))
        maps.append(m)
    return maps


def kernel(**inputs):
    nc = build()
    maps = make_in_maps(inputs, list(range(8)))
    res = run_bass_kernel_spmd(nc, maps, core_ids=list(range(8)))
    return np.stack([r["out"] for r in res.results], axis=0).astype(np.float32)
```

```python
import os
import threading
import numpy as np
from contextlib import ExitStack
import concourse.bass as bass
import concourse.mybir as mybir
from concourse.bass_utils import run_bass_kernel_spmd

F32 = mybir.dt.float32
BF16 = mybir.dt.bfloat16
I32 = mybir.dt.int32
U32 = mybir.dt.uint32
AF = mybir.ActivationFunctionType
ALU = mybir.AluOpType

S = 4096
D = 1024
NT = S // 128
INW = 2320
NE = 256
NBLK = 512
ALPHA = 2.0 ** 0.25
EPS = 1e-5
BIG = 1.0e30
NDS = 40

C_ID, C_TRI, C_BO, C_LS, C_ON, C_MP, C_MC = 0, 128, 256, 384, 512, 640, 768
C_IOTA = 896
C_BV = 1152
C_INVF = 1156
C_PI = 1188
NCONST = 1189


class Buf:
    def __init__(self, t):
        self.t = t
        self.w = {}
        self.r = {}

    def __getitem__(self, k):
        return self.t[k]


class Acc:
    def __init__(self, b):
        self.b = b


class Sch:
    def __init__(self, nc, st):
        self.nc = nc
        self.eng = dict(pe=nc.tensor, dve=nc.vector, act=nc.scalar, pool=nc.gpsimd, sp=nc.sync)
        self.csem = {k: st.enter_context(nc.semaphore("c_" + k)) for k in ["pe", "dve", "act", "pool"]}
        self.ccnt = {k: 0 for k in self.csem}
        self.dsems = [st.enter_context(nc.semaphore("d%d" % i)) for i in range(NDS)]
        self.dval = [0] * NDS
        self.dnext = 0
        self.waited = {k: {} for k in self.eng}
        self.n_inst = 0
        self.il = None

    def _wait(self, e, ev):
        sem, val, src = ev
        key = id(sem)
        if self.waited[e].get(key, 0) >= val:
            return
        self.eng[e].wait_ge(sem, val)
        self.waited[e][key] = val

    def _deps(self, e, R, W, acc):
        for b in R:
            for ev in b.w.values():
                if not (e == "pe" and ev[2] == "pe"):
                    self._wait(e, ev)
        for b in W:
            a_ = acc
            if isinstance(b, Acc):
                a_, b = True, b.b
            if not a_:
                for ev in b.w.values():
                    if not (e == "pe" and ev[2] == "pe"):
                        self._wait(e, ev)
            for ev in b.r.values():
                if not (e == "pe" and ev[2] == "pe"):
                    self._wait(e, ev)

    def _upd(self, ev, R, W, acc):
        key = id(ev[0])
        for b in R:
            b.r[key] = ev
        for b in W:
            a_ = acc
            if isinstance(b, Acc):
                a_, b = True, b.b
            if a_:
                b.w[key] = ev
            else:
                b.w = {key: ev}
            b.r = {}

    def op(self, e, fn, R=(), W=(), acc=False):
        self._deps(e, R, W, acc)
        inst = fn()
        self.ccnt[e] += 1
        inst.then_inc(self.csem[e], 1)
        self._upd((self.csem[e], self.ccnt[e], e), R, W, acc)
        self.n_inst += 1
        self._yield()

    def dma(self, q, fn, R=(), W=(), acc=False):
        i = self.dnext
        self.dnext = (i + 1) % NDS
        if self.dval[i] > 0:
            self._wait(q, (self.dsems[i], self.dval[i], "dma"))
        self._deps(q, R, W, acc)
        inst = fn()
        self.dval[i] += 16
        inst.then_inc(self.dsems[i], 16)
        self._upd((self.dsems[i], self.dval[i], "dma"), R, W, acc)
        self.n_inst += 1
        self._yield()

    def barrier(self):
        for e in self.eng:
            for k, sem in self.csem.items():
                if self.ccnt[k] > 0 and not (e == "pe" and k == "pe"):
                    self._wait(e, (sem, self.ccnt[k], k))
            for i, sem in enumerate(self.dsems):
                if self.dval[i] > 0:
                    self._wait(e, (sem, self.dval[i], "dma"))

    def _yield(self):
        il = self.il
        if il is None:
            return
        me = threading.current_thread().name
        names = il["names"]
        k = names.index(me)
        for d in range(1, len(names)):
            o = names[(k + d) % len(names)]
            if not il["done"][o]:
                il["sem"][o].release()
                il["sem"][me].acquire()
                return

    def interleave(self, *fs):
        names = ["S%d" % i for i in range(len(fs))]
        il = dict(names=names, sem={n: threading.Semaphore(0) for n in names}, done={n: False for n in names}, exc=[])
        self.il = il

        def body(name, f):
            il["sem"][name].acquire()
            try:
                f()
            except BaseException as e:
                il["exc"].append(e)
            il["done"][name] = True
            k = names.index(name)
            for d in range(1, len(names)):
                o = names[(k + d) % len(names)]
                if not il["done"][o]:
                    il["sem"][o].release()
                    break

        ths = [threading.Thread(target=body, args=(n, f), name=n) for n, f in zip(names, fs)]
        for t in ths:
            t.start()
        il["sem"][names[0]].release()
        for t in ths:
            t.join()
        self.il = None
        if il["exc"]:
            raise il["exc"][0]

    def finish(self, bufs):
        for b in bufs:
            for ev in b.w.values():
                self._wait("sp", ev)


def build(debug=False):
    nc = bass.Bass("TRN2", target_bir_lowering=False)
    dt_in = lambda n, s, d=F32: nc.dram_tensor(n, s, d, kind="ExternalInput").ap()
    x_d = dt_in("x", [S, D])
    c_d = dt_in("c", [128, 8])
    pos_d = dt_in("pos", [128, NT], I32)
    consts_d = dt_in("consts", [128, NCONST])
    wada_d = dt_in("w_ada", [D, 6 * D])
    bada_d = dt_in("b_ada", [1, 6 * D])
    win_d = dt_in("w_in", [D, INW])
    bin_d = dt_in("b_in", [1, INW])
    bgkT_d = dt_in("b_gklo", [16, 1])
    sinks_d = dt_in("sinks", [1, 8])
    wgk2_d = dt_in("w_gk2", [16, 256])
    bgk2_d = dt_in("b_gk2", [1, 256])
    gnorm_d = dt_in("gnorm", [1, 512])
    wo_d = dt_in("w_o", [D, D])
    bo_d = dt_in("b_o", [1, D])
    ln1g_d = dt_in("ln1_g", [1, D])
    ln1b_d = dt_in("ln1_b", [1, D])
    wr_d = dt_in("w_router", [D, NE])
    rb_d = dt_in("router_bias", [1, NE])
    ne_rows = 128 if (debug and not os.environ.get('K_FULL')) else NE * 128
    weg_d = dt_in("w_exp_gate", [ne_rows, 2048])
    weu_d = dt_in("w_exp_up", [ne_rows, 2048])
    wed_d = dt_in("w_exp_down", [ne_rows, 2048])
    wsg_d = dt_in("w_sh_gate", [D, 256])
    wsu_d = dt_in("w_sh_up", [D, 256])
    wsd_d = dt_in("w_sh_down", [256, D])
    ln2g_d = dt_in("ln2_g", [1, D])
    ln2b_d = dt_in("ln2_b", [1, D])
    out_d = nc.dram_tensor("out", [S, D], F32, kind="ExternalOutput").ap()
    dbg_d = nc.dram_tensor("dbg", [8, 128, D], F32, kind="ExternalOutput").ap() if debug else None
    dbg16_d = nc.dram_tensor("dbg16", [4, 128, D], BF16, kind="ExternalOutput").ap() if debug else None
    z_s = nc.dram_tensor("z_s", [S, D], F32, kind="ExternalOutput" if debug else "Internal").ap()
    h2_s = nc.dram_tensor("h2_s", [S, D], BF16, kind="ExternalOutput" if debug else "Internal").ap()
    pf_s = nc.dram_tensor("pf_s", [S, NE], F32, kind="Internal").ap()
    xdisp = nc.dram_tensor("xdisp", [NBLK * 128, D], BF16, kind="Internal").ap()
    ydisp = nc.dram_tensor("ydisp", [NBLK * 128, D], BF16, kind="Internal").ap()

    with ExitStack() as st:
        sc = Sch(nc, st)
        V, A, P, PE = nc.vector, nc.scalar, nc.gpsimd, nc.tensor

        def sb(stack, name, shape, dt=F32):
            return Buf(stack.enter_context(nc.sbuf_tensor(name, shape, dt)))

        def ps(stack, name, shape, dt=F32):
            return Buf(stack.enter_context(nc.psum_tensor(name, shape, dt)))

        B_pf = Buf(None); B_out = Buf(None); B_z = Buf(None); B_h2s = Buf(None); B_xd = Buf(None); B_yd = Buf(None); B_dbg = Buf(None)

        cst = sb(st, "cst", [128, NCONST])
        cb16 = sb(st, "cb16", [128, 896], BF16)
        modv = sb(st, "modv", [128, 6 * D])
        idxf_t = sb(st, "idxf_t", [128, NT, 8])
        w_t = sb(st, "w_t", [128, NT, 8])
        cntb = sb(st, "cntb", [128, NE])
        st_ps = ExitStack()
        ps_tr = ps(st_ps, "ps_tr", [128, 1024], BF16)
        pm = [ps(st_ps, "pm%d" % i, [128, 512]) for i in range(6)]
        ps_tr2 = ps(st_ps, "ps_tr2", [128, 1024], BF16)

        ident = lambda: cst[:, C_ID:C_ID + 128]
        id16 = lambda: cb16[:, 0:128]

        dq = ["sp", "act"]
        sc.dma("sp", lambda: nc.sync.dma_start(out=cst[:], in_=consts_d[:, :]), W=[cst])
        sc.op("dve", lambda: V.tensor_copy(cb16[:], cst[:, 0:896]), R=[cst], W=[cb16])
        sc.op("pool", lambda: P.memset(cntb[:], 0.0), W=[cntb])

        def ln_stats(stk_tiles, src, R):
            stats, mv, rstd, nmr = stk_tiles
            sc.op("dve", lambda: V.bn_stats(stats[:, 0:6], src[:, 0:512]), R=R, W=[stats])
            sc.op("dve", lambda: V.bn_stats(stats[:, 6:12], src[:, 512:1024]), R=R, W=[stats], acc=True)
            sc.op("dve", lambda: V.bn_aggr(mv[:], stats[:]), R=[stats], W=[mv])
            sc.op("act", lambda: A.activation(rstd[:], mv[:, 1:2], AF.Ln, bias=EPS, scale=1.0), R=[mv], W=[rstd])
            sc.op("act", lambda: A.activation(rstd[:], rstd[:], AF.Exp, scale=-0.5), R=[rstd], W=[rstd])
            sc.op("dve", lambda: V.tensor_scalar(nmr[:], mv[:, 0:1], rstd[:, 0:1], -1.0, ALU.mult, ALU.mult), R=[mv, rstd], W=[nmr])
            return mv, rstd

        with ExitStack() as s0:
            c8 = sb(s0, "c8", [128, 8]); ca8 = sb(s0, "ca8", [128, 8])
            cbT = sb(s0, "cbT", [128, 8, 128], BF16)
            wst = [sb(s0, "wada%d" % i, [128, 8, 512]) for i in range(2)]
            wst16 = [sb(s0, "wada16_%d" % i, [128, 8, 512], BF16) for i in range(2)]
            bada = sb(s0, "bada", [128, 6 * D])
            sc.dma("sp", lambda: nc.sync.dma_start(out=c8[:], in_=c_d[:, :]), W=[c8])
            sc.dma("act", lambda: A.dma_start(out=bada[:], in_=bada_d.partition_broadcast(128)), W=[bada])
            sc.op("act", lambda: A.activation(ca8[:], c8[:], AF.Silu), R=[c8], W=[ca8])
            for j in range(8):
                sc.op("dve", lambda j=j: V.tensor_scalar(cbT[:, j, :], cst[:, C_ON:C_ON + 128], ca8[:, j:j + 1], None,
                                                         ALU.mult), R=[ca8, cst], W=[cbT], acc=(j > 0))
            wv = wada_d.rearrange("(p j) n -> p j n", j=8)
            for n in range(12):
                w = wst[n % 2]
                sc.dma(dq[n % 2], lambda n=n, w=w: sc.eng[dq[n % 2]].dma_start(out=w[:], in_=wv[:, :, n * 512:(n + 1) * 512]), W=[w])
                p = pm[n % 2]
                w16 = wst16[n % 2]
                if n % 2 == 0:
                    sc.op("act", lambda w=w, w16=w16: A.copy(w16[:], w[:]), R=[w], W=[w16])
                else:
                    sc.op("dve", lambda w=w, w16=w16: V.tensor_copy(w16[:], w[:]), R=[w], W=[w16])
                for j in range(8):
                    sc.op("pe", lambda j=j, w16=w16, p=p: PE.matmul(p[:], cbT[:, j, :], w16[:, j, :], start=(j == 0), stop=(j == 7)),
                          R=[cbT, w16], W=[p], acc=(j > 0))
                sc.op("dve", lambda n=n, p=p: V.tensor_tensor(modv[:, n * 512:(n + 1) * 512], p[:], bada[:, n * 512:(n + 1) * 512],
                                                              ALU.add), R=[p, bada], W=[modv], acc=True)
            for sec in (1, 2, 4, 5):
                sc.op("dve", lambda sec=sec: V.tensor_scalar(modv[:, sec * D:(sec + 1) * D], modv[:, sec * D:(sec + 1) * D], 1.0,
                                                             None, ALU.add), R=[modv], W=[modv])
        sc.barrier()
        SH1, SC1, G1, SH2, SC2, G2 = [lambda k=k: modv[:, k * D:(k + 1) * D] for k in range(6)]

        with ExitStack() as sa:
            win16 = sb(sa, "win16", [128, 8, INW], BF16)
            wo16 = sb(sa, "wo16", [128, 8, D], BF16)
            wr16 = sb(sa, "wr16", [128, 8, NE], BF16)
            wsgu16 = sb(sa, "wsgu16", [128, 8, 512], BF16)
            wsd16 = sb(sa, "wsd16", [128, 2, D], BF16)
            wgk2 = sb(sa, "wgk2", [16, 256]); bgkT = sb(sa, "bgkT", [16, 1])
            binb = sb(sa, "binb", [128, INW], BF16); bgk2b = sb(sa, "bgk2b", [128, 256]); gnb = sb(sa, "gnb", [128, 512])
            bog1 = sb(sa, "bog1", [128, D]); ln1g = sb(sa, "ln1g", [128, D]); ln1b = sb(sa, "ln1b", [128, D])
            rbb = sb(sa, "rbb", [128, NE]); esink = sb(sa, "esink", [128, 8])
            cosT = sb(sa, "cosT", [128, NT, 32], BF16); sinT = sb(sa, "sinT", [128, NT, 32], BF16)
            with ExitStack() as sl:
                stg = [sb(sl, "stg%d" % i, [128, 8, 512]) for i in range(2)]
                k = 0
                win_v = win_d.rearrange("(j p) c -> p j c", p=128)
                jobs = []
                for c0 in range(0, INW, 512):
                    jobs.append((win_v, c0, min(512, INW - c0), win16, c0, 8))
                wo_v = wo_d.rearrange("(j p) c -> p j c", p=128)
                for c0 in range(0, D, 512):
                    jobs.append((wo_v, c0, 512, wo16, c0, 8))
                jobs.append((wr_d.rearrange("(p j) c -> p j c", j=8), 0, 256, wr16, 0, 8))
                jobs.append((wsg_d.rearrange("(p j) c -> p j c", j=8), 0, 256, wsgu16, 0, 8))
                jobs.append((wsu_d.rearrange("(p j) c -> p j c", j=8), 0, 256, wsgu16, 256, 8))
                wsd_v = wsd_d.rearrange("(p j) c -> p j c", j=2)
                for c0 in range(0, D, 512):
                    jobs.append((wsd_v, c0, 512, wsd16, c0, 2))
                for (src, c0, cw, dst, d0, nj) in jobs:
                    s_ = stg[k % 2]
                    q = dq[k % 2]
                    sc.dma(q, lambda s_=s_, src=src, c0=c0, cw=cw, nj=nj, q=q: sc.eng[q].dma_start(
                        out=s_[:, 0:nj, 0:cw], in_=src[:, :, c0:c0 + cw]), W=[s_])
                    e = "dve" if k % 2 == 0 else "act"
                    if e == "dve":
                        sc.op("dve", lambda s_=s_, dst=dst, d0=d0, cw=cw, nj=nj: V.tensor_copy(dst[:, 0:nj, d0:d0 + cw], s_[:, 0:nj, 0:cw]),
                              R=[s_], W=[dst], acc=True)
                    else:
                        sc.op("act", lambda s_=s_, dst=dst, d0=d0, cw=cw, nj=nj: A.copy(dst[:, 0:nj, d0:d0 + cw], s_[:, 0:nj, 0:cw]),
                              R=[s_], W=[dst], acc=True)
                    k += 1
                bst = stg[0][:].rearrange("p j c -> p (j c)")[:, 0:INW]
                sc.dma("sp", lambda: nc.sync.dma_start(out=bst, in_=bin_d.partition_broadcast(128)), W=[stg[0]])
                sc.op("dve", lambda: V.tensor_copy(binb[:], bst), R=[stg[0]], W=[binb])
                for (dst, src) in ((bgk2b, bgk2_d), (gnb, gnorm_d), (bog1, bo_d), (ln1g, ln1g_d), (ln1b, ln1b_d),
                                   (rbb, rb_d), (esink, sinks_d)):
                    sc.dma("sp", lambda dst=dst, src=src: nc.sync.dma_start(out=dst[:], in_=src.partition_broadcast(128)), W=[dst])
                sc.dma("sp", lambda: nc.sync.dma_start(out=wgk2[:], in_=wgk2_d[:, :]), W=[wgk2])
                sc.dma("sp", lambda: nc.sync.dma_start(out=bgkT[:], in_=bgkT_d[:, :]), W=[bgkT])
                sc.op("act", lambda: A.activation(esink[:], esink[:], AF.Exp), R=[esink], W=[esink])
                sc.op("dve", lambda: V.tensor_tensor(bog1[:], bog1[:], G1(), ALU.mult), R=[bog1, modv], W=[bog1])
                posi = sb(sl, "posi", [128, NT], I32); posf = sb(sl, "posf", [128, NT])
                ang = sb(sl, "ang", [128, NT, 32]); fr = sb(sl, "fr", [128, NT, 32]); ki = sb(sl, "ki", [128, NT, 32], I32)
                kf = sb(sl, "kf", [128, NT, 32]); m1 = sb(sl, "m1", [128, NT, 32])
                sc.dma("sp", lambda: nc.sync.dma_start(out=posi[:], in_=pos_d[:, :]), W=[posi])
                sc.op("dve", lambda: V.tensor_copy(posf[:], posi[:]), R=[posi], W=[posf])
                for i in range(NT):
                    sc.op("dve", lambda i=i: V.tensor_scalar(ang[:, i, :], cst[:, C_INVF:C_INVF + 32], posf[:, i:i + 1], None, ALU.mult),
                          R=[posf, cst], W=[ang], acc=(i > 0))
                for (tab, off) in ((sinT, 0.5), (cosT, 0.75)):
                    sc.op("dve", lambda off=off: V.tensor_scalar(fr[:], ang[:], 1.0 / (2 * np.pi), off, ALU.mult, ALU.add), R=[ang], W=[fr])
                    sc.op("dve", lambda: V.tensor_copy(ki[:], fr[:]), R=[fr], W=[ki])
                    sc.op("dve", lambda: V.tensor_copy(kf[:], ki[:]), R=[ki], W=[kf])
                    sc.op("dve", lambda: V.tensor_tensor(fr[:], fr[:], kf[:], ALU.subtract), R=[fr, kf], W=[fr])
                    sc.op("dve", lambda: V.tensor_scalar(m1[:], fr[:], 0.0, None, ALU.is_lt), R=[fr], W=[m1])
                    sc.op("dve", lambda: V.tensor_tensor(fr[:], fr[:], m1[:], ALU.add), R=[fr, m1], W=[fr])
                    sc.op("dve", lambda: V.tensor_scalar(m1[:], fr[:], 1.0, None, ALU.is_ge), R=[fr], W=[m1])
                    sc.op("dve", lambda: V.tensor_tensor(fr[:], fr[:], m1[:], ALU.subtract), R=[fr, m1], W=[fr])
                    sc.op("dve", lambda: V.tensor_scalar(fr[:], fr[:], 2 * np.pi, -np.pi, ALU.mult, ALU.add), R=[fr], W=[fr])
                    sc.op("dve", lambda: V.tensor_scalar(fr[:], fr[:], 3.141592, -3.141592, ALU.min, ALU.max), R=[fr], W=[fr])
                    sc.op("act", lambda tab=tab: A.activation(tab[:], fr[:], AF.Sin), R=[fr], W=[tab])

                sc1T = sb(sl, "sc1T", [128, 8]); sh1T = sb(sl, "sh1T", [128, 8]); shb = sb(sl, "shb", [128, 8, 128], BF16)
                for (dstT, sec) in ((sc1T, 1), (sh1T, 0)):
                    for j in range(8):
                        sc.op("pe", lambda j=j, sec=sec: PE.transpose(pm[0][:, 0:128], modv[:, sec * D + j * 128:sec * D + (j + 1) * 128], ident()), R=[modv, cst], W=[pm[0]])
                        sc.op("act", lambda j=j, dstT=dstT: A.copy(dstT[:, j:j + 1], pm[0][:, 0:1]), R=[pm[0]], W=[Acc(dstT)] if j > 0 else [dstT])
                for j in range(8):
                    sc.op("dve", lambda j=j: V.tensor_scalar(shb[:, j, :], cst[:, C_ON:C_ON + 128], sh1T[:, j:j + 1], None, ALU.mult), R=[sh1T, cst], W=[Acc(shb)] if j > 0 else [shb])
                gi = 0
                for c0 in range(0, INW, 512):
                    cw = min(512, INW - c0)
                    p = pm[1 + gi % 2]
                    for j in range(8):
                        sc.op("pe", lambda j=j, p=p, c0=c0, cw=cw: PE.matmul(p[:, 0:cw], shb[:, j, :], win16[:, j, c0:c0 + cw], start=(j == 0), stop=(j == 7)),
                              R=[shb, win16], W=[p], acc=(j > 0))
                    sc.op("dve", lambda p=p, c0=c0, cw=cw: V.tensor_tensor(binb[:, c0:c0 + cw], p[:, 0:cw], binb[:, c0:c0 + cw], ALU.add), R=[p, binb], W=[binb])
                    gi += 1
                gkb = sb(sl, "gkb", [128, 128])
                sc.op("dve", lambda: V.memset(gkb[:], 0.0), W=[gkb])
                sc.op("dve", lambda: V.tensor_copy(gkb[:, 0:16], binb[:, 2304:2320]), R=[binb], W=[gkb])
                sc.op("pe", lambda: PE.transpose(pm[0][:, 0:128], gkb[:], ident()), R=[gkb, cst], W=[pm[0]])
                sc.op("act", lambda: A.copy(bgkT[:], pm[0][0:16, 0:1]), R=[pm[0]], W=[bgkT])
                for j in range(8):
                    sc.op("dve", lambda j=j: V.tensor_scalar(win16[:, j, :], win16[:, j, :], sc1T[:, j:j + 1], None, ALU.mult), R=[win16, sc1T], W=[win16])
                    sc.op("dve", lambda j=j: V.tensor_tensor(wo16[:, j, :], wo16[:, j, :], G1(), ALU.mult), R=[wo16, modv], W=[wo16])
                for j in range(2):
                    sc.op("dve", lambda j=j: V.tensor_tensor(wsd16[:, j, :], wsd16[:, j, :], G2(), ALU.mult), R=[wsd16, modv], W=[wsd16])
            sc.barrier()
            xt = sb(sa, "xt", [128, D]); t32a = sb(sa, "t32a", [128, D]); t32b = sb(sa, "t32b", [128, D])
            stats = sb(sa, "stats", [128, 12]); mv = sb(sa, "mv", [128, 2]); rstd = sb(sa, "rstd", [128, 1])
            stats1 = sb(sa, "stats1", [128, 12]); mv1 = sb(sa, "mv1", [128, 2]); rstd1 = sb(sa, "rstd1", [128, 1]); nmr1 = sb(sa, "nmr1", [128, 1]); nmr = sb(sa, "nmr", [128, 1])
            h16 = sb(sa, "h16", [128, D], BF16); hT = sb(sa, "hT", [128, 8, 128], BF16)
            projs = [sb(sa, "proj", [128, INW], BF16), Buf(modv.t[:, 0:1160].bitcast(BF16))]
            sglb = Buf(modv.t[:, 1160:1800])
            qkr = sb(sa, "qkr", [128, 640], BF16)
            qT = sb(sa, "qT", [128, 512], BF16)
            kT = [sb(sa, "kT%d" % i, [128, 128], BF16) for i in range(2)]
            vext = [sb(sa, "vext%d" % i, [128, 2, 65], BF16) for i in range(2)]
            ecur = sb(sa, "ecur", [128, 512], BF16); eprev = sb(sa, "eprev", [128, 512], BF16)
            mp4 = sb(sa, "mp4", [128, 512], BF16); mc4 = sb(sa, "mc4", [128, 512], BF16); tri4 = sb(sa, "tri4", [128, 512], BF16)
            den = sb(sa, "den", [128, 8])
            mixeds = [sb(sa, "mixed%d" % i_, [128, D], BF16) for i_ in range(3)]; resids = [sb(sa, "resid%d" % i_, [128, D], BF16) for i_ in range(3)]; mixT = sb(sa, "mixT", [128, 8, 128], BF16)
            gkTs = [sb(sa, "gkT%d" % i_, [16, 128]) for i_ in range(2)]; zg = sb(sa, "zg", [128, 256]); lg = sb(sa, "lg", [128, 256])
            bsb = sb(sa, "bsb", [128, 256]); eb = sb(sa, "eb", [128, 256]); enb = sb(sa, "enb", [128, 256]); ebl = sb(sa, "ebl", [128, 256])
            qin = sb(sa, "qin", [128, 320], BF16); kin = sb(sa, "kin", [128, 320], BF16); kout = sb(sa, "kout", [128, 256], BF16)
            vl16 = sb(sa, "vl16", [128, 512], BF16)
            qkT = sb(sa, "qkT", [128, 8, 128], BF16)
            aT = sb(sa, "aT", [128, 512], BF16)
            dec = sb(sa, "dec", [128, 8])
            S32 = sb(sa, "S32", [128, 4, 128]); S16a = sb(sa, "S16a", [128, 4, 128], BF16); S16b = sb(sa, "S16b", [128, 4, 128], BF16)
            blx = sb(sa, "blx", [128, 320])
            qTA = sb(sa, "qTA", [128, 4, 128], BF16); qTB = sb(sa, "qTB", [128, 4, 128], BF16)
            ssq = sb(sa, "ssq", [128, 4]); sgl = sglb
            h2p = sb(sa, "h2p", [128, D], BF16); h2T = sb(sa, "h2T", [128, 8, 128], BF16)
            scores, biased, masked, sel, gw, posfull = [sb(sa, "rs%d" % i_, [128, NE]) for i_ in range(6)]
            oneh = sb(sa, "oneh", [128, NE])
            g8 = sb(sa, "g8", [128, 8, 8]); gs = sb(sa, "gs", [128, 8]); gtop = sb(sa, "gtop", [128, 8]); gmask = sb(sa, "gmask", [128, 8])
            pen = sb(sa, "pen", [128, 8]); top8 = sb(sa, "top8", [128, 8]); idx8 = sb(sa, "idx8", [128, 8], U32)
            sumw = sb(sa, "sumw", [128, 1])
            sg = oneh; actp = sb(sa, "actp", [128, 256], BF16); actT = sb(sa, "actT", [128, 2, 128], BF16)

            for r in range(4):
                sc.op("pool", lambda r=r: P.tensor_copy(mp4[:, r * 128:(r + 1) * 128], cb16[:, C_MP:C_MP + 128]), R=[cb16], W=[mp4], acc=True)
                sc.op("pool", lambda r=r: P.tensor_copy(mc4[:, r * 128:(r + 1) * 128], cb16[:, C_MC:C_MC + 128]), R=[cb16], W=[mc4], acc=True)
                sc.op("pool", lambda r=r: P.tensor_copy(tri4[:, r * 128:(r + 1) * 128], cb16[:, C_TRI:C_TRI + 128]), R=[cb16], W=[tri4], acc=True)
            sc.op("pool", lambda: P.memset(S32[:], 0.0), W=[S32])
            sc.op("pool", lambda: P.memset(qTA[:], 0.0), W=[qTA])
            sc.op("pool", lambda: P.memset(blx[:], 0.0), W=[blx])
            sc.op("pool", lambda: P.memset(qin[:], 0.0), W=[qin])
            sc.op("pool", lambda: P.memset(kin[:], 0.0), W=[kin])
            sc.op("pool", lambda: P.memset(qTB[:], 0.0), W=[qTB])
            sc.op("pool", lambda: P.memset(S16a[:], 0.0), W=[S16a])
            for b_ in vext:
                sc.op("pool", lambda b_=b_: P.memset(b_[:], 1.0), W=[b_])

            psA = Buf(ps_tr.t[:, 0:512]); psB = Buf(ps_tr.t[:, 512:1024])

            def transposes(src, n, dst_ap, dstbuf, pst=None):
                pst = ps_tr if pst is None else pst
                for j in range(n):
                    sc.op("pe", lambda j=j: PE.transpose(pst[:, j * 128:(j + 1) * 128], src[:, j * 128:(j + 1) * 128], id16()),
                          R=[src, cb16], W=[pst], acc=(j > 0))
                sc.op("act", lambda: A.copy(dst_ap, pst[:, 0:n * 128]), R=[pst], W=[dstbuf])

            ntile = int(os.environ.get("K_NTILE", NT))
            kstop = float(os.environ.get("K_STOP", 99))
            def seg_a(i):
                cur, prv = i % 2, (i + 1) % 2
                r0 = i * 128
                mixed = mixeds[i % 3]; resid = resids[i % 3]; proj = projs[i % 2]; gkT = gkTs[i % 2]
                sc.dma("sp", lambda: nc.sync.dma_start(out=xt[:], in_=x_d[r0:r0 + 128, :]), W=[xt])
                ln_stats((stats1, mv1, rstd1, nmr1), xt, [xt])
                sc.op("dve", lambda: V.scalar_tensor_tensor(resid[:], xt[:], ALPHA, bog1[:], ALU.mult, ALU.add), R=[xt, bog1], W=[resid])
                sc.op("act", lambda: A.activation(h16[:], xt[:], AF.Identity, bias=nmr1[:, 0:1], scale=rstd1[:, 0:1]), R=[xt, nmr1, rstd1], W=[h16])
                for rr_ in range(2):
                    for j in range(4):
                        jj = rr_ * 4 + j
                        sc.op("pe", lambda j=j, jj=jj: PE.transpose(psA[:, j * 128:(j + 1) * 128], h16[:, jj * 128:(jj + 1) * 128], id16()), R=[h16, cb16], W=[psA], acc=(j > 0))
                    sc.op("act", lambda rr_=rr_: A.copy(hT[:, rr_ * 4:rr_ * 4 + 4, :].rearrange("p j t -> p (j t)"), psA[:, 0:512]), R=[psA], W=[Acc(hT)] if rr_ else [hT])

            def seg_b(i):
                cur, prv = i % 2, (i + 1) % 2
                r0 = i * 128
                mixed = mixeds[i % 3]; resid = resids[i % 3]; proj = projs[i % 2]; gkT = gkTs[i % 2]
                gi = 0
                for c0 in range(0, INW, 512):
                    cw = min(512, INW - c0)
                    p = pm[gi % 2]
                    for j in range(8):
                        sc.op("pe", lambda j=j, p=p, c0=c0, cw=cw: PE.matmul(p[:, 0:cw], hT[:, j, :], win16[:, j, c0:c0 + cw], start=(j == 0), stop=(j == 7)),
                              R=[hT, win16], W=[p], acc=(j > 0))
                    sc.op("dve", lambda p=p, c0=c0, cw=cw: V.tensor_tensor(proj[:, c0:c0 + cw], p[:, 0:cw], binb[:, c0:c0 + cw], ALU.add),
                          R=[p, binb], W=[proj], acc=True)
                    gi += 1
                for j in range(8):
                    sc.op("pe", lambda j=j: PE.matmul(pm[1][0:16, 0:128], win16[:, j, 2304:2320], hT[:, j, :], start=(j == 0), stop=(j == 7)),
                          R=[hT, win16], W=[pm[1]], acc=(j > 0))
                sc.op("dve", lambda: V.tensor_scalar(gkT[:], pm[1][0:16, 0:128], bgkT[:, 0:1], None, ALU.add), R=[pm[1], bgkT], W=[gkT])

            def seg_c(i):
                cur, prv = i % 2, (i + 1) % 2
                r0 = i * 128
                mixed = mixeds[i % 3]; resid = resids[i % 3]; proj = projs[i % 2]; gkT = gkTs[i % 2]
                qk = proj[:, 0:640].rearrange("p (h two d) -> p h two d", two=2, d=32)
                qo = qkr[:].rearrange("p (h two d) -> p h two d", two=2, d=32)
                cB = cosT[:, i, :].unsqueeze(1).broadcast_to([128, 10, 32])
                sB = sinT[:, i, :].unsqueeze(1).broadcast_to([128, 10, 32])
                sc.op("dve", lambda: V.tensor_tensor(xt[:, 0:320].rearrange("p (h d) -> p h d", d=32), qk[:, :, 0, :], cB, ALU.mult), R=[proj, cosT], W=[xt])
                sc.op("pool", lambda: P.tensor_tensor(xt[:, 320:640].rearrange("p (h d) -> p h d", d=32), qk[:, :, 1, :], sB, ALU.mult), R=[proj, sinT], W=[xt])
                sc.op("dve", lambda: V.tensor_tensor(qo[:, :, 0, :], xt[:, 0:320].rearrange("p (h d) -> p h d", d=32), xt[:, 320:640].rearrange("p (h d) -> p h d", d=32), ALU.subtract), R=[xt], W=[qkr])
                sc.op("dve", lambda: V.tensor_tensor(xt[:, 0:320].rearrange("p (h d) -> p h d", d=32), qk[:, :, 1, :], cB, ALU.mult), R=[proj, cosT], W=[xt])
                sc.op("pool", lambda: P.tensor_tensor(xt[:, 320:640].rearrange("p (h d) -> p h d", d=32), qk[:, :, 0, :], sB, ALU.mult), R=[proj, sinT], W=[xt])
                sc.op("dve", lambda: V.tensor_tensor(qo[:, :, 1, :], xt[:, 0:320].rearrange("p (h d) -> p h d", d=32), xt[:, 320:640].rearrange("p (h d) -> p h d", d=32), ALU.add), R=[xt], W=[qkr], acc=True)
                for j in range(4):
                    sc.op("pe", lambda j=j: PE.transpose(psA[:, j * 128:(j + 1) * 128], qkr[:, j * 128:(j + 1) * 128], id16()), R=[qkr, cb16], W=[psA], acc=(j > 0))
                sc.op("act", lambda: A.copy(qT[:], psA[:, 0:512]), R=[psA], W=[qT])
                sc.op("pe", lambda: PE.transpose(psA[:, 0:128], qkr[:, 512:640], id16()), R=[qkr, cb16], W=[psA])
                sc.op("act", lambda: A.copy(kT[cur][:], psA[:, 0:128]), R=[psA], W=[kT[cur]])
                sc.op("pool", lambda: P.tensor_copy(vext[cur][:, :, 0:64], proj[:, 640:768].rearrange("p (g d) -> p g d", d=64)),
                      R=[proj], W=[vext[cur]])

            def seg_d(i):
                cur, prv = i % 2, (i + 1) % 2
                r0 = i * 128
                mixed = mixeds[i % 3]; resid = resids[i % 3]; proj = projs[i % 2]; gkT = gkTs[i % 2]
                for g in range(2):
                    pr = slice(g * 64, (g + 1) * 64)
                    sc.op("pe", lambda: PE.matmul(pm[0][:], kT[cur][pr, :], qT[pr, :], start=True, stop=True), R=[kT[cur], qT], W=[pm[0]])
                    sc.op("act", lambda: A.activation(ecur[:], pm[0][:], AF.Exp, scale=0.125), R=[pm[0]], W=[ecur])
                    sc.op("pool", lambda: P.tensor_tensor(ecur[:], ecur[:], mc4[:], ALU.mult), R=[ecur, mc4], W=[ecur])
                    if i > 0:
                        sc.op("pe", lambda: PE.matmul(pm[0][:], kT[prv][pr, :], qT[pr, :], start=True, stop=True), R=[kT[prv], qT], W=[pm[0]])
                        sc.op("act", lambda: A.activation(eprev[:], pm[0][:], AF.Exp, scale=0.125), R=[pm[0]], W=[eprev])
                        sc.op("pool", lambda: P.tensor_tensor(eprev[:], eprev[:], mp4[:], ALU.mult), R=[eprev, mp4], W=[eprev])
                    po = pm[1]
                    for c in range(4):
                        oc = slice(c * 65, (c + 1) * 65)
                        if i > 0:
                            sc.op("pe", lambda c=c, oc=oc: PE.matmul(po[:, oc], eprev[:, c * 128:(c + 1) * 128], vext[prv][:, g, :], start=True, stop=False),
                                  R=[eprev, vext[prv]], W=[po], acc=(c > 0))
                        sc.op("pe", lambda c=c, oc=oc: PE.matmul(po[:, oc], ecur[:, c * 128:(c + 1) * 128], vext[cur][:, g, :], start=(i == 0), stop=True),
                              R=[ecur, vext[cur]], W=[po], acc=(c > 0 or i > 0))
                    pov = po[:, 0:260].rearrange("p (c e) -> p c e", e=65)
                    sc.op("dve", lambda: V.tensor_tensor(den[:, g * 4:(g + 1) * 4], pov[:, :, 64], esink[:, g * 4:(g + 1) * 4], ALU.add),
                          R=[po, esink], W=[den])
                    sc.op("dve", lambda: V.reciprocal(den[:, g * 4:(g + 1) * 4], den[:, g * 4:(g + 1) * 4]), R=[den], W=[den])
                    for c in range(4):
                        h = g * 4 + c
                        sc.op("act", lambda c=c, h=h: A.activation(mixed[:, h * 64:(h + 1) * 64], po[:, c * 65:c * 65 + 64], AF.Copy, scale=den[:, h:h + 1]),
                              R=[po, den], W=[mixed], acc=True)

            def seg_e(i):
                cur, prv = i % 2, (i + 1) % 2
                r0 = i * 128
                mixed = mixeds[i % 3]; resid = resids[i % 3]; proj = projs[i % 2]; gkT = gkTs[i % 2]
                sc.op("pe", lambda: PE.matmul(pm[2][:, 0:256], gkT[:], wgk2[:], start=True, stop=True), R=[gkT, wgk2], W=[pm[2]])
                sc.op("dve", lambda: V.tensor_tensor(zg[:], pm[2][:, 0:256], bgk2b[:], ALU.add), R=[pm[2], bgk2b], W=[zg])
                sc.op("act", lambda: A.activation(zg[:], zg[:], AF.Exp, scale=-1.0), R=[zg], W=[zg])
                sc.op("act", lambda: A.activation(zg[:], zg[:], AF.Ln, bias=1.0, scale=1.0), R=[zg], W=[zg])
                sc.op("dve", lambda: V.tensor_scalar(lg[:], zg[:], -1.0 / 16.0, None, ALU.mult), R=[zg], W=[lg])
                sc.op("pe", lambda: PE.matmul(pm[3][:, 0:256], cst[:, C_TRI:C_TRI + 128], lg[:], start=True, stop=True), R=[cst, lg], W=[pm[3]])
                sc.op("pe", lambda: PE.matmul(pm[3][:, 256:512], cst[:, C_BO:C_BO + 128], lg[:], start=True, stop=True), R=[cst, lg], W=[pm[3]], acc=True)
                sc.op("act", lambda: A.copy(blx[:, 0:256], pm[3][:, 256:512]), R=[pm[3]], W=[blx])
                for hd in range(4):
                    sc.op("pe", lambda hd=hd: PE.transpose(pm[2][:, hd * 128:(hd + 1) * 128], blx[:, hd * 64:hd * 64 + 128], ident()),
                          R=[blx, cst], W=[pm[2]], acc=(hd > 0))
                pT = pm[2][0:64, :].rearrange("p (h t) -> p h t", t=128)
                for ch in range(2):
                    sc.op("act", lambda ch=ch: A.activation(dec[0:64, ch * 4:ch * 4 + 4], pT[:, :, ch * 64], AF.Exp), R=[pm[2]], W=[dec], acc=(ch > 0))
                sc.op("act", lambda: A.copy(bsb[:], pm[3][:, 0:256]), R=[pm[3]], W=[bsb])
                sc.op("act", lambda: A.activation(eb[:], bsb[:], AF.Exp), R=[bsb], W=[eb])
                sc.op("act", lambda: A.activation(enb[:], bsb[:], AF.Exp, scale=-1.0), R=[bsb], W=[enb])
                sc.op("dve", lambda: V.tensor_tensor(ebl[:], pm[3][:, 256:512], bsb[:], ALU.subtract), R=[pm[3], bsb], W=[ebl])
                sc.op("act", lambda: A.activation(ebl[:], ebl[:], AF.Exp), R=[ebl], W=[ebl])
                sc.op("dve", lambda: V.scalar_tensor_tensor(qin[:, 0:256], proj[:, 768:1024], 0.125, eb[:], ALU.mult, ALU.mult), R=[proj, eb], W=[qin])
                sc.op("pool", lambda: P.tensor_tensor(kin[:, 0:256], proj[:, 1024:1280], enb[:], ALU.mult), R=[proj, enb], W=[kin])
                sc.op("pool", lambda: P.tensor_tensor(kout[:], proj[:, 1024:1280], ebl[:], ALU.mult), R=[proj, ebl], W=[kout])
                sc.op("pool", lambda: P.tensor_copy(vl16[:], proj[:, 1280:1792]), R=[proj], W=[vl16])
                for hd in range(4):
                    sc.op("pe", lambda hd=hd: PE.transpose(psB[:, hd * 128:(hd + 1) * 128], qin[:, hd * 64:hd * 64 + 128], id16()), R=[qin, cb16], W=[psB], acc=(hd > 0))
                sc.op("act", lambda: A.copy(qkT[0:64, 0:4, :].rearrange("p j t -> p (j t)"), psB[0:64, 0:512]), R=[psB], W=[qkT])
                pq = psB[0:64, 0:512].rearrange("p (h t) -> p h t", t=128)
                sc.op("act", lambda: A.copy(qTA[0:64, :, 0:64], pq[:, :, 0:64]), R=[psB], W=[qTA])
                sc.op("act", lambda: A.copy(qTB[0:64, :, 64:128], pq[:, :, 64:128]), R=[psB], W=[qTB])
                for hd in range(4):
                    sc.op("pe", lambda hd=hd: PE.transpose(psB[:, hd * 128:(hd + 1) * 128], kin[:, hd * 64:hd * 64 + 128], id16()), R=[kin, cb16], W=[psB], acc=(hd > 0))
                sc.op("act", lambda: A.copy(qkT[0:64, 4:8, :].rearrange("p j t -> p (j t)"), psB[0:64, 0:512]), R=[psB], W=[Acc(qkT)])
                for hd in range(4):
                    sc.op("pe", lambda hd=hd: PE.matmul(pm[3][:, hd * 128:(hd + 1) * 128], qkT[0:64, 4 + hd, :], qkT[0:64, hd, :], start=True, stop=True),
                          R=[qkT], W=[pm[3]], acc=(hd > 0))
                sc.op("dve", lambda: V.tensor_tensor(aT[:], pm[3][:], tri4[:], ALU.mult), R=[pm[3], tri4], W=[aT])
                pu = pm[2]

                def u_and_state(ch, Sdst):
                    rr = slice(ch * 64, (ch + 1) * 64)
                    for hd in range(4):
                        sc.op("pe", lambda hd=hd: PE.matmul(pu[0:64, hd * 128:(hd + 1) * 128], kout[rr, hd * 64:(hd + 1) * 64],
                                                            vl16[rr, hd * 128:(hd + 1) * 128], start=True, stop=True),
                              R=[kout, vl16], W=[pu], acc=(hd > 0))
                    for hd in range(4):
                        sc.op("dve", lambda hd=hd: V.scalar_tensor_tensor(S32[0:64, hd, :], S32[0:64, hd, :], dec[0:64, ch * 4 + hd:ch * 4 + hd + 1],
                                                                          pu[0:64, hd * 128:(hd + 1) * 128], ALU.mult, ALU.add),
                              R=[S32, dec, pu], W=[S32], acc=(hd > 0))
                    sc.op("act", lambda: A.copy(Sdst[0:64], S32[0:64]), R=[S32], W=[Sdst])

                u_and_state(0, S16b)
                pg = pm[3]
                for hd in range(4):
                    oc = slice(hd * 128, (hd + 1) * 128)
                    sc.op("pe", lambda hd=hd, oc=oc: PE.matmul(pg[:, oc], aT[:, hd * 128:(hd + 1) * 128], vl16[:, oc], start=True, stop=False),
                          R=[aT, vl16], W=[pg], acc=(hd > 0))
                    sc.op("pe", lambda hd=hd, oc=oc: PE.matmul(pg[:, oc], qTA[0:64, hd, :], S16a[0:64, hd, :], start=False, stop=False),
                          R=[qTA, S16a], W=[pg], acc=True)
                    sc.op("pe", lambda hd=hd, oc=oc: PE.matmul(pg[:, oc], qTB[0:64, hd, :], S16b[0:64, hd, :], start=False, stop=True),
                          R=[qTB, S16b], W=[pg], acc=True)
                u_and_state(1, S16a)
                for hd in range(4):
                    sc.op("act", lambda hd=hd: A.activation(sglb[:, 512:640], pg[:, hd * 128:(hd + 1) * 128], AF.Square, accum_out=ssq[:, hd:hd + 1]),
                          R=[pg], W=[sglb, Acc(ssq)] if hd > 0 else [sglb, ssq])
                sc.op("act", lambda: A.activation(ssq[:], ssq[:], AF.Ln, bias=EPS, scale=1.0 / 128.0), R=[ssq], W=[ssq])
                sc.op("act", lambda: A.activation(ssq[:], ssq[:], AF.Exp, scale=-0.5), R=[ssq], W=[ssq])
                sc.op("act", lambda: A.activation(sgl[:, 0:512], proj[:, 1792:2304], AF.Silu), R=[proj], W=[sgl])
                sc.op("pool", lambda: P.tensor_tensor(sgl[:, 0:512], sgl[:, 0:512], gnb[:], ALU.mult), R=[sgl, gnb], W=[sgl])
                for hd in range(4):
                    sc.op("dve", lambda hd=hd: V.scalar_tensor_tensor(mixed[:, 512 + hd * 128:512 + (hd + 1) * 128], pg[:, hd * 128:(hd + 1) * 128], ssq[:, hd:hd + 1],
                                                                      sgl[:, hd * 128:(hd + 1) * 128], ALU.mult, ALU.mult),
                          R=[pg, ssq, sgl], W=[mixed], acc=True)
                if debug and i == 0:
                    sc.dma("sp", lambda: nc.sync.dma_start(out=dbg16_d[0, :, :], in_=mixed[:]), R=[mixed], W=[B_dbg], acc=True)
                    sc.dma("sp", lambda: nc.sync.dma_start(out=dbg16_d[1, :, 0:640], in_=qkr[:]), R=[qkr], W=[B_dbg], acc=True)
                    sc.dma("sp", lambda: nc.sync.dma_start(out=dbg16_d[2, :, 0:512], in_=ecur[:]), R=[ecur], W=[B_dbg], acc=True)
                    sc.dma("sp", lambda: nc.sync.dma_start(out=dbg16_d[2, :, 512:1024], in_=qT[:]), R=[qT], W=[B_dbg], acc=True)
                    sc.dma("sp", lambda: nc.sync.dma_start(out=dbg16_d[3, :, 0:128], in_=kT[cur][:]), R=[kT[cur]], W=[B_dbg], acc=True)
                    sc.dma("sp", lambda: nc.sync.dma_start(out=dbg16_d[3, :, 128:258], in_=vext[cur][:].rearrange("p g e -> p (g e)")), R=[vext[cur]], W=[B_dbg], acc=True)
                    sc.dma("sp", lambda: nc.sync.dma_start(out=dbg_d[3, :, 0:8], in_=den[:]), R=[den], W=[B_dbg], acc=True)
                    sc.dma("sp", lambda: nc.sync.dma_start(out=dbg_d[4, :, 0:256], in_=lg[:]), R=[lg], W=[B_dbg], acc=True)
                    sc.dma("sp", lambda: nc.sync.dma_start(out=dbg_d[5, :, 0:256], in_=bsb[:]), R=[bsb], W=[B_dbg], acc=True)
                    sc.dma("sp", lambda: nc.sync.dma_start(out=dbg_d[6, :, 0:8], in_=dec[:]), R=[dec], W=[B_dbg], acc=True)
                    sc.dma("sp", lambda: nc.sync.dma_start(out=dbg_d[6, :, 8:12], in_=ssq[:]), R=[ssq], W=[B_dbg], acc=True)

            def seg_f(i):
                cur, prv = i % 2, (i + 1) % 2
                r0 = i * 128
                mixed = mixeds[i % 3]; resid = resids[i % 3]; proj = projs[i % 2]; gkT = gkTs[i % 2]
                transposes(mixed, 8, mixT[:].rearrange("p j t -> p (j t)"), mixT, pst=ps_tr2)
                for hf in range(2):
                    p = pm[4 + hf]
                    for j in range(8):
                        sc.op("pe", lambda j=j, p=p, hf=hf: PE.matmul(p[:], mixT[:, j, :], wo16[:, j, hf * 512:(hf + 1) * 512], start=(j == 0), stop=(j == 7)),
                              R=[mixT, wo16], W=[p], acc=(j > 0))
                    sc.op("dve", lambda p=p, hf=hf: V.tensor_tensor(t32a[:, hf * 512:(hf + 1) * 512], p[:], resid[:, hf * 512:(hf + 1) * 512], ALU.add),
                          R=[p, resid], W=[t32a], acc=(hf > 0))
                ln_stats((stats, mv, rstd, nmr), t32a, [t32a])
                sc.op("act", lambda: A.activation(t32a[:], t32a[:], AF.Identity, bias=nmr[:, 0:1], scale=rstd[:, 0:1]), R=[t32a, nmr, rstd], W=[t32a])
                sc.op("dve", lambda: V.tensor_tensor(t32a[:], t32a[:], ln1g[:], ALU.mult), R=[t32a, ln1g], W=[t32a])
                sc.op("dve", lambda: V.tensor_tensor(t32a[:], t32a[:], ln1b[:], ALU.add), R=[t32a, ln1b], W=[t32a])
                if debug and i == 0:
                    sc.dma("sp", lambda: nc.sync.dma_start(out=dbg_d[0, :, :], in_=t32a[:]), R=[t32a], W=[B_dbg], acc=True)
                ln_stats((stats, mv, rstd, nmr), t32a, [t32a])
                sc.op("act", lambda: A.activation(t32b[:], t32a[:], AF.Identity, bias=nmr[:, 0:1], scale=rstd[:, 0:1]), R=[t32a, nmr, rstd], W=[t32b])
                sc.op("dve", lambda: V.tensor_tensor(t32b[:], t32b[:], SC2(), ALU.mult), R=[t32b, modv], W=[t32b])
                sc.op("dve", lambda: V.tensor_tensor(t32b[:], t32b[:], SH2(), ALU.add), R=[t32b, modv], W=[t32b])
                sc.op("act", lambda: A.copy(h2p[:].rearrange("t (j p) -> t p j", p=128), t32b[:].rearrange("t (p j) -> t p j", j=8)), R=[t32b], W=[h2p])
                sc.dma("act", lambda: A.dma_start(out=h2_s[r0:r0 + 128, :], in_=h2p[:]), R=[h2p], W=[B_h2s], acc=True)
                transposes(h2p, 8, h2T[:].rearrange("p j t -> p (j t)"), h2T, pst=ps_tr2)

            def seg_g(i):
                cur, prv = i % 2, (i + 1) % 2
                r0 = i * 128
                mixed = mixeds[i % 3]; resid = resids[i % 3]; proj = projs[i % 2]; gkT = gkTs[i % 2]
                for j in range(8):
                    sc.op("pe", lambda j=j: PE.matmul(pm[4][:, 0:256], h2T[:, j, :], wr16[:, j, :], start=(j == 0), stop=(j == 7)), R=[h2T, wr16], W=[pm[4]], acc=(j > 0))
                for j in range(8):
                    sc.op("pe", lambda j=j: PE.matmul(pm[5][:], h2T[:, j, :], wsgu16[:, j, :], start=(j == 0), stop=(j == 7)), R=[h2T, wsgu16], W=[pm[5]], acc=(j > 0))
                sc.op("act", lambda: A.activation(scores[:], pm[4][:, 0:256], AF.Sigmoid), R=[pm[4]], W=[scores])
                sc.op("act", lambda: A.activation(sg[:], pm[5][:, 0:256], AF.Silu), R=[pm[5]], W=[sg])
                sc.op("dve", lambda: V.tensor_tensor(actp[:].rearrange("t (j p) -> t p j", p=128), sg[:].rearrange("t (p j) -> t p j", j=2),
                                                     pm[5][:, 256:512].rearrange("t (p j) -> t p j", j=2), ALU.mult), R=[sg, pm[5]], W=[actp])
                transposes(actp, 2, actT[:].rearrange("p j t -> p (j t)"), actT, pst=ps_tr2)
                for hf in range(2):
                    p = pm[4 + hf]
                    for j in range(2):
                        sc.op("pe", lambda j=j, p=p, hf=hf: PE.matmul(p[:], actT[:, j, :], wsd16[:, j, hf * 512:(hf + 1) * 512], start=(j == 0), stop=(j == 1)),
                              R=[actT, wsd16], W=[p], acc=(j > 0))
                    sc.op("dve", lambda p=p, hf=hf: V.scalar_tensor_tensor(t32b[:, hf * 512:(hf + 1) * 512], t32a[:, hf * 512:(hf + 1) * 512], ALPHA, p[:], ALU.mult, ALU.add),
                          R=[p, t32a], W=[t32b], acc=(hf > 0))
                sc.dma("sp", lambda: nc.sync.dma_start(out=z_s[r0:r0 + 128, :], in_=t32b[:]), R=[t32b], W=[B_z], acc=True)

            def seg_h(i):
                cur, prv = i % 2, (i + 1) % 2
                r0 = i * 128
                mixed = mixeds[i % 3]; resid = resids[i % 3]; proj = projs[i % 2]; gkT = gkTs[i % 2]
                sc.op("dve", lambda: V.tensor_tensor(biased[:], scores[:], rbb[:], ALU.add), R=[scores, rbb], W=[biased])
                for g in range(8):
                    sc.op("dve", lambda g=g: V.max(out=g8[:, g, :], in_=biased[:, g * 32:(g + 1) * 32]), R=[biased], W=[g8], acc=(g > 0))
                sc.op("dve", lambda: V.tensor_tensor(gs[:], g8[:, :, 0], g8[:, :, 1], ALU.add), R=[g8], W=[gs])
                sc.op("dve", lambda: V.max(out=gtop[:], in_=gs[:]), R=[gs], W=[gtop])
                sc.op("dve", lambda: V.tensor_scalar(gmask[:], gs[:], gtop[:, 3:4], None, ALU.is_ge), R=[gs, gtop], W=[gmask])
                sc.op("dve", lambda: V.tensor_scalar(pen[:], gmask[:], BIG, -BIG, ALU.mult, ALU.add), R=[gmask], W=[pen])
                b3 = biased[:].rearrange("p (g e) -> p g e", e=32)
                m3 = masked[:].rearrange("p (g e) -> p g e", e=32)
                sc.op("dve", lambda: V.tensor_tensor(m3, b3, gmask[:].unsqueeze(2).broadcast_to([128, 8, 32]), ALU.mult), R=[biased, gmask], W=[masked])
                sc.op("dve", lambda: V.tensor_tensor(m3, m3, pen[:].unsqueeze(2).broadcast_to([128, 8, 32]), ALU.add), R=[masked, pen], W=[masked])
                sc.op("dve", lambda: V.max(out=top8[:], in_=masked[:]), R=[masked], W=[top8])
                sc.op("dve", lambda: V.tensor_scalar(sel[:], masked[:], top8[:, 7:8], None, ALU.is_ge), R=[masked, top8], W=[sel])
                sc.op("dve", lambda: V.scalar_tensor_tensor(gw[:], sel[:], 1.0, scores[:], ALU.mult, ALU.mult, accum_out=sumw[:]), R=[sel, scores], W=[gw, sumw])
                sc.op("dve", lambda: V.reciprocal(sumw[:], sumw[:]), R=[sumw], W=[sumw])
                sc.op("dve", lambda: V.tensor_scalar(gw[:], gw[:], sumw[:, 0:1], 2.5, ALU.mult, ALU.mult), R=[gw, sumw], W=[gw])
                sc.op("dve", lambda: V.max(out=w_t[:, i, :], in_=gw[:]), R=[gw], W=[Acc(w_t)])
                sc.op("dve", lambda: V.max_index(out=idx8[:], in_max=w_t[:, i, :], in_values=gw[:]), R=[w_t, gw], W=[idx8])
                sc.op("dve", lambda: V.tensor_copy(idxf_t[:, i, :], idx8[:]), R=[idx8], W=[Acc(idxf_t)])
                sc.op("pe", lambda: PE.matmul(pm[4][:, 0:256], cst[:, C_LS:C_LS + 128], sel[:], start=True, stop=True), R=[cst, sel], W=[pm[4]])
                sc.op("pe", lambda: PE.matmul(pm[4][:, 256:512], cst[:, C_ON:C_ON + 128], sel[:], start=True, stop=True), R=[cst, sel], W=[pm[4]], acc=True)
                sc.op("dve", lambda: V.tensor_tensor(posfull[:], pm[4][:, 0:256], cntb[:], ALU.add), R=[pm[4], cntb], W=[posfull])
                sc.op("dve", lambda: V.tensor_tensor(cntb[:], pm[4][:, 256:512], cntb[:], ALU.add), R=[pm[4], cntb], W=[cntb])
                sc.dma("sp", lambda: nc.sync.dma_start(out=pf_s[r0:r0 + 128, :], in_=posfull[:]), R=[posfull], W=[B_pf], acc=True)

            def P1(i):
                seg_a(i); seg_b(i); seg_c(i); seg_d(i)

            def P3(i):
                seg_f(i); seg_g(i); seg_h(i)

            for step in range(ntile + 2):
                fs = []
                if step < ntile:
                    fs.append(lambda step=step: P1(step))
                if 0 <= step - 1 < ntile:
                    fs.append(lambda step=step: seg_e(step - 1))
                if 0 <= step - 2 < ntile:
                    fs.append(lambda step=step: P3(step - 2))
                if len(fs) == 1:
                    fs[0]()
                else:
                    sc.interleave(*fs)
        sc.barrier()
        if debug and not os.environ.get('K_FULL'):
            sc.finish([B_dbg, B_z, B_h2s])
            return nc
        IOA = bass.IndirectOffsetOnAxis
        slot_t = sb(st, "slot_t", [128, NT, 8], I32)
        slot_v = [Buf(slot_t.t[:, i_, :]) for i_ in range(NT)]
        idxw = sb(st, "idxw", [128, NBLK], I32)
        nblk = int(os.environ.get("K_NBLK", NBLK))
        with ExitStack() as sbk:
            cnti = sb(sbk, "cnti", [128, NE], I32); padf = sb(sbk, "padf", [128, NE]); pend = sb(sbk, "pend", [128, NE])
            pstart = sb(sbk, "pstart", [128, NE]); ones256 = sb(sbk, "ones256", [128, NE]); jkb = sb(sbk, "jkb", [128, NE])
            onehb = sb(sbk, "onehb", [128, NE])
            blk = sb(sbk, "blk", [128, 4]); D4 = sb(sbk, "D4", [128, 4, 128]); tmpf = sb(sbk, "tmpf", [128, NBLK])
            psk = sb(sbk, "psk", [128, NT, 8]); sltf = sb(sbk, "sltf", [128, NT, 8])
            h2b = [sb(sbk, "h2b%d" % i, [128, D], BF16) for i in range(2)]
            sc.op("pool", lambda: P.memset(ones256[:], 1.0), W=[ones256])
            sc.op("dve", lambda: V.tensor_copy(cnti[:], cntb[:]), R=[cntb], W=[cnti])
            sc.op("dve", lambda: V.tensor_scalar(cnti[:], cnti[:], 127, None, ALU.add), R=[cnti], W=[cnti])
            sc.op("dve", lambda: V.tensor_scalar(cnti[:], cnti[:], 7, None, ALU.arith_shift_right), R=[cnti], W=[cnti])
            sc.op("dve", lambda: V.tensor_scalar(cnti[:], cnti[:], 7, None, ALU.logical_shift_left), R=[cnti], W=[cnti])
            sc.op("dve", lambda: V.tensor_copy(padf[:], cnti[:]), R=[cnti], W=[padf])
            sc.op("dve", lambda: V.tensor_tensor_scan(pend[:], ones256[:], padf[:], 0.0, ALU.mult, ALU.add), R=[ones256, padf], W=[pend])
            sc.op("dve", lambda: V.tensor_tensor(pstart[:], pend[:], padf[:], ALU.subtract), R=[pend, padf], W=[pstart])
            for j in range(4):
                sc.op("dve", lambda j=j: V.tensor_scalar(jkb[:], pend[:], cst[:, C_BV + j:C_BV + j + 1], 0.0, ALU.is_le, ALU.add, accum_out=blk[:, j:j + 1]),
                      R=[pend, cst], W=[jkb, Acc(blk)] if j > 0 else [jkb, blk])
            sc.op("dve", lambda: V.tensor_scalar(blk[:], blk[:], 255.0, None, ALU.min), R=[blk], W=[blk])
            for j in range(4):
                sc.op("dve", lambda j=j: V.tensor_scalar(D4[:, j, :], ident(), blk[:, j:j + 1], None, ALU.mult), R=[cst, blk], W=[D4], acc=(j > 0))
            sc.op("pe", lambda: PE.matmul(pm[0][:], cst[:, C_ON:C_ON + 128], D4[:].rearrange("p j t -> p (j t)"), start=True, stop=True), R=[cst, D4], W=[pm[0]])
            bef = sb(sbk, "bef", [128, NBLK]); chg = sb(sbk, "chg", [128, NBLK])
            sc.op("act", lambda: A.copy(bef[:], pm[0][:]), R=[pm[0]], W=[bef])
            sc.op("pool", lambda: P.memset(chg[:], 0.0), W=[chg])
            sc.op("dve", lambda: V.tensor_tensor(chg[:, 1:NBLK], bef[:, 1:NBLK], bef[:, 0:NBLK - 1], ALU.is_equal), R=[bef, chg], W=[chg])
            for qq in range(4):
                sc.op("dve", lambda qq=qq: V.memset(chg[:, qq * (NBLK // 4):qq * (NBLK // 4) + 1], 0.0), R=[chg], W=[chg])
            sc.op("dve", lambda: V.tensor_scalar(tmpf[:], bef[:], 128.0, cst[:, C_PI:C_PI + 1], ALU.mult, ALU.add), R=[bef, cst], W=[tmpf])
            sc.op("dve", lambda: V.scalar_tensor_tensor(tmpf[:], chg[:], 16777216.0, tmpf[:], ALU.mult, ALU.add), R=[chg, tmpf], W=[tmpf])
            sc.op("dve", lambda: V.tensor_copy(idxw[:], tmpf[:]), R=[tmpf], W=[idxw])
            pfb = [sb(sbk, "pfb%d" % i_, [128, NE]) for i_ in range(2)]
            for i in range(ntile):
                pf = pfb[i % 2]
                sc.dma("act", lambda: A.dma_start(out=pf[:], in_=pf_s[i * 128:(i + 1) * 128, :]), R=[B_pf], W=[pf])
                sc.op("dve", lambda: V.tensor_tensor(pf[:], pf[:], pstart[:], ALU.add), R=[pf, pstart], W=[pf])
                for k in range(8):
                    sc.op("dve", lambda k=k: V.tensor_scalar(onehb[:], cst[:, C_IOTA:C_IOTA + 256], idxf_t[:, i, k:k + 1], None, ALU.is_equal), R=[cst, idxf_t], W=[onehb])
                    sc.op("dve", lambda k=k: V.scalar_tensor_tensor(jkb[:], onehb[:], 1.0, pf[:], ALU.mult, ALU.mult, accum_out=psk[:, i, k:k + 1]),
                          R=[onehb, pf], W=[jkb, Acc(psk)])
                sc.op("dve", lambda: V.tensor_copy(slot_v[i][:, :], psk[:, i, :]), R=[psk], W=[slot_v[i]])
                hb = h2b[i % 2]
                sc.dma("sp", lambda: nc.sync.dma_start(out=hb[:], in_=h2_s[i * 128:(i + 1) * 128, :]), R=[B_h2s], W=[hb])
                for k in range(8):
                    sc.dma("pool", lambda k=k: P.indirect_dma_start(out=xdisp, out_offset=IOA(ap=slot_v[i][:, k:k + 1], axis=0), in_=hb[:], in_offset=None),
                           R=[hb, slot_v[i]], W=[B_xd], acc=True)
        sc.barrier()
        st_ps.close()
        NS = 4
        with ExitStack() as scx:
            ptx = [ps(scx, "ptx%d" % i, [128, 1024], BF16) for i in range(2)]
            pta = ps(scx, "pta", [128, 1024], BF16)
            pgus = [ps(scx, "pgu%d" % i, [128, 512]) for i in range(2)]
            pds = [ps(scx, "pd%d" % i, [128, 512]) for i in range(2)]
            wg16 = [sb(scx, "wg16_%d" % i, [128, 2048], BF16) for i in range(NS)]
            wu16 = [sb(scx, "wu16_%d" % i, [128, 2048], BF16) for i in range(NS)]
            wd16 = [sb(scx, "wd16_%d" % i, [128, 2048], BF16) for i in range(NS)]
            xb = [sb(scx, "xb%d" % i, [128, D], BF16) for i in range(NS)]
            xT = [sb(scx, "xT%d" % i, [128, 8, 128], BF16) for i in range(2)]
            acT = [sb(scx, "acT%d" % i, [128, 2, 128], BF16) for i in range(2)]
            y32 = [sb(scx, "y32_%d" % i, [128, D], BF16) for i in range(2)]
            sgc = [sb(scx, "sgc%d" % i, [128, 256]) for i in range(2)]; acp = [sb(scx, "acp%d" % i, [128, 256], BF16) for i in range(2)]
            bof = lambda t: t // NS + (NBLK // NS) * (t % NS)
            bnd_reg = P.to_reg(NE * 128 - 1)

            def loads(t):
                p = t % NS
                bb = bof(t)
                ix = idxw[:, bb:bb + 1]
                for (dst, src) in ((wg16[p], weg_d), (wu16[p], weu_d), (wd16[p], wed_d)):
                    sc.dma("pool", lambda dst=dst, src=src: P.indirect_dma_start(out=dst[:], out_offset=None, in_=src, in_offset=IOA(ap=ix, axis=0),
                                                                                 bounds_check=bnd_reg, oob_is_err=False), R=[idxw], W=[dst])
                sc.dma("sp", lambda: nc.sync.dma_start(out=xb[p][:], in_=xdisp[bb * 128:(bb + 1) * 128, :]), R=[B_xd], W=[xb[p]])

            def fa(t):
                p, q = t % NS, t % 2
                for j in range(8):
                    sc.op("pe", lambda j=j: PE.transpose(ptx[q][:, j * 128:(j + 1) * 128], xb[p][:, j * 128:(j + 1) * 128], id16()), R=[xb[p], cb16], W=[ptx[q]], acc=(j > 0))
                sc.op("act", lambda: A.copy(xT[q][:].rearrange("p j t -> p (j t)"), ptx[q][:, :]), R=[ptx[q]], W=[xT[q]])

            def fb(t):
                p, q = t % NS, t % 2
                pgu = pgus[q]
                for (w16, c0) in ((wg16[p], 0), (wu16[p], 256)):
                    for j in range(8):
                        sc.op("pe", lambda j=j, w16=w16, c0=c0: PE.matmul(pgu[:, c0:c0 + 256], xT[q][:, j, :], w16[:, j * 256:(j + 1) * 256], start=(j == 0), stop=(j == 7)),
                              R=[xT[q], w16], W=[pgu], acc=(j > 0 or c0 > 0))
                sc.op("act", lambda: A.activation(sgc[q][:], pgu[:, 0:256], AF.Silu), R=[pgu], W=[sgc[q]])
                sc.op("dve", lambda: V.tensor_tensor(acp[q][:].rearrange("t (j p) -> t p j", p=128), sgc[q][:].rearrange("t (p j) -> t p j", j=2),
                                                     pgu[:, 256:512].rearrange("t (p j) -> t p j", j=2), ALU.mult), R=[sgc[q], pgu], W=[acp[q]])

            def fc(t):
                q = t % 2
                for j in range(2):
                    sc.op("pe", lambda j=j: PE.transpose(pta[:, j * 128:(j + 1) * 128], acp[q][:, j * 128:(j + 1) * 128], id16()), R=[acp[q], cb16], W=[pta], acc=(j > 0))
                sc.op("act", lambda: A.copy(acT[q][:].rearrange("p j t -> p (j t)"), pta[:, 0:256]), R=[pta], W=[acT[q]])

            def back(t):
                p, q = t % NS, t % 2
                bb = bof(t)
                for hf in range(2):
                    pp = pds[hf]
                    for j in range(2):
                        sc.op("pe", lambda j=j, pp=pp, hf=hf: PE.matmul(pp[:], acT[q][:, j, :], wd16[p][:, j * 1024 + hf * 512:j * 1024 + (hf + 1) * 512], start=(j == 0), stop=(j == 1)),
                              R=[acT[q], wd16[p]], W=[pp], acc=(j > 0))
                sc.op("act", lambda: A.copy(y32[q][:, 0:512], pds[0][:]), R=[pds[0]], W=[y32[q]])
                sc.op("dve", lambda: V.tensor_copy(y32[q][:, 512:1024], pds[1][:]), R=[pds[1]], W=[y32[q]], acc=True)
                sc.dma("act", lambda: A.dma_start(out=ydisp[bb * 128:(bb + 1) * 128, :], in_=y32[q][:]), R=[y32[q]], W=[B_yd], acc=True)

            for t in range(min(NS - 1, nblk)):
                loads(t)
            fa(0)
            for t in range(nblk):
                fb(t)
                if t + 1 < nblk:
                    fa(t + 1)
                if t >= 1:
                    back(t - 1)
                fc(t)
                if t + NS - 1 < nblk:
                    loads(t + NS - 1)
            back(nblk - 1)
        sc.barrier()
        with ExitStack() as sd:
            ln2g = sb(sd, "ln2g", [128, D]); ln2b = sb(sd, "ln2b", [128, D])
            zts = [sb(sd, "zt%d" % i, [128, D]) for i in range(2)]; accs = [sb(sd, "accb%d" % i, [128, D]) for i in range(2)]
            yg = [sb(sd, "yg%d" % i, [128, D], BF16) for i in range(16)]
            pacc = [[ps(sd, "pacc%d_%d" % (a_, h_), [128, 512]) for h_ in range(2)] for a_ in range(2)]
            dg = [sb(sd, "dg%d" % i_, [128, 8, 128], BF16) for i_ in range(2)]
            stats = sb(sd, "stats2", [128, 12]); mv = sb(sd, "mv2", [128, 2]); rstd = sb(sd, "rstd2", [128, 1]); nmr2 = sb(sd, "nmr2", [128, 1])
            sc.dma("sp", lambda: nc.sync.dma_start(out=ln2g[:], in_=ln2g_d.partition_broadcast(128)), W=[ln2g])
            sc.dma("sp", lambda: nc.sync.dma_start(out=ln2b[:], in_=ln2b_d.partition_broadcast(128)), W=[ln2b])
            for i in range(ntile):
                zt, accb = zts[i % 2], accs[i % 2]
                sc.dma("sp", lambda: nc.sync.dma_start(out=zt[:], in_=z_s[i * 128:(i + 1) * 128, :]), R=[B_z], W=[zt])
                for k in range(8):
                    g_ = yg[(i % 2) * 8 + k]
                    sc.dma("pool", lambda: P.indirect_dma_start(out=g_[:], out_offset=None, in_=ydisp, in_offset=IOA(ap=slot_v[i][:, k:k + 1], axis=0)),
                           R=[B_yd, slot_v[i]], W=[g_])
                dgi = dg[i % 2]
                for k in range(8):
                    sc.op("act", lambda k=k: A.activation(dgi[:, k, :], id16(), AF.Copy, scale=w_t[:, i, k:k + 1]), R=[cb16, w_t], W=[Acc(dgi)] if k > 0 else [dgi])
                for hf in range(2):
                    pa = pacc[i % 2][hf]
                    for k in range(8):
                        g_ = yg[(i % 2) * 8 + k]
                        sc.op("pe", lambda k=k, g_=g_, pa=pa, hf=hf: PE.matmul(pa[:], dgi[:, k, :], g_[:, hf * 512:(hf + 1) * 512], start=(k == 0), stop=(k == 7)),
                              R=[dgi, g_], W=[pa], acc=(k > 0))
                    sc.op("dve", lambda pa=pa, hf=hf: V.tensor_tensor(accb[:, hf * 512:(hf + 1) * 512], pa[:], modv[:, 5 * D + hf * 512:5 * D + (hf + 1) * 512], ALU.mult),
                          R=[pa, modv], W=[accb], acc=(hf > 0))
                sc.op("dve", lambda: V.tensor_tensor(accb[:], accb[:], zt[:], ALU.add), R=[accb, zt], W=[accb])
                ln_stats((stats, mv, rstd, nmr2), accb, [accb])
                sc.op("act", lambda: A.activation(accb[:], accb[:], AF.Identity, bias=nmr2[:, 0:1], scale=rstd[:, 0:1]), R=[accb, nmr2, rstd], W=[accb])
                sc.op("dve", lambda: V.tensor_tensor(accb[:], accb[:], ln2g[:], ALU.mult), R=[accb, ln2g], W=[accb])
                sc.op("dve", lambda: V.tensor_tensor(accb[:], accb[:], ln2b[:], ALU.add), R=[accb, ln2b], W=[accb])
                sc.dma("sp", lambda: nc.sync.dma_start(out=out_d[i * 128:(i + 1) * 128, :], in_=accb[:]), R=[accb], W=[B_out], acc=True)
        sc.finish([B_out])
    return nc


def make_consts():
    c = np.zeros((128, NCONST), np.float32)
    p = np.arange(128)
    c[:, C_ID:C_ID + 128] = np.eye(128)
    same = (p[:, None] // 64) == (p[None, :] // 64)
    c[:, C_TRI:C_TRI + 128] = (same & (p[:, None] <= p[None, :]))
    c[:, C_BO:C_BO + 128] = same
    c[:, C_LS:C_LS + 128] = (p[:, None] < p[None, :])
    c[:, C_ON:C_ON + 128] = 1.0
    c[:, C_MP:C_MP + 128] = (p[:, None] > p[None, :])
    c[:, C_MC:C_MC + 128] = (p[:, None] <= p[None, :])
    c[:, C_IOTA:C_IOTA + 256] = np.arange(256)[None, :]
    for j in range(4):
        c[:, C_BV + j] = 128.0 * (j * 128 + p)
    half = 32
    c[:, C_INVF:C_INVF + 32] = (10000.0 ** (-np.arange(half, dtype=np.float32) / half)).astype(np.float32)[None, :]
    c[:, C_PI] = p
    return c


QPERM = np.concatenate([np.arange(h * 64, (h + 1) * 64) for h in (0, 4, 1, 5, 2, 6, 3, 7)])


def make_in_maps(inputs, cores):
    f = lambda a: np.ascontiguousarray(a, dtype=np.float32)
    colperm = np.concatenate([QPERM, np.arange(512, INW)])
    w_in = f(inputs["w_in"][0][:, colperm])
    b_in = f(inputs["b_in"][0][colperm])
    shared = dict(
        consts=make_consts(), w_ada=f(inputs["w_ada"][0]), b_ada=f(inputs["b_ada"][0][None]), w_in=w_in, b_in=b_in[None],
        b_gklo=f(b_in[2304:2320].reshape(16, 1)), sinks=f(inputs["attn_sinks"][0][None]), w_gk2=f(inputs["w_gk2"][0]),
        b_gk2=f(inputs["b_gk2"][0][None]), gnorm=f(np.tile(inputs["gla_norm_g"][0], 4)[None]), w_o=f(inputs["w_o"][0]),
        b_o=f(inputs["b_o"][0][None]), ln1_g=f(inputs["ln1_g"][0][None]), ln1_b=f(inputs["ln1_b"][0][None]),
        w_router=f(inputs["w_router"][0]), router_bias=f(inputs["router_bias"][0][None]),
        w_exp_gate=f(inputs["w_exp_gate"][0]).reshape(-1, 2048), w_exp_up=f(inputs["w_exp_up"][0]).reshape(-1, 2048),
        w_exp_down=f(inputs["w_exp_down"][0]).reshape(-1, 2048),
        w_sh_gate=f(inputs["w_sh_gate"][0]), w_sh_up=f(inputs["w_sh_up"][0]), w_sh_down=f(inputs["w_sh_down"][0]),
        ln2_g=f(inputs["ln2_g"][0][None]), ln2_b=f(inputs["ln2_b"][0][None]),
    )
    maps = []
    for b in cores:
        m = dict(shared)
        m["x"] = f(inputs["x"][b])
        m["c"] = f(inputs["c"][b].reshape(128, 8))
        m["pos"] = np.ascontiguousarray(inputs["positions"][b].reshape(NT, 128).T.astype(np.int32))
        maps.append(m)
    return maps


def kernel(**inputs):
    nc = build()
    maps = make_in_maps(inputs, list(range(8)))
    res = run_bass_kernel_spmd(nc, maps, core_ids=list(range(8)))
    return np.stack([r["out"] for r in res.results], axis=0).astype(np.float32)
```
